# Optimizing a Trainium2 kernel written in Bass

```python
import math
import jax, jax.numpy as jnp
from jax import lax
import numpy as np

D_MODEL = 2048
BATCH = 4
SEQ = 2048
DEPTH = 1

GRID_W = 64
CTX_LEN = 256
MIX_WIDTH = D_MODEL
POOL_WIDTH = MIX_WIDTH // 2
POOL_WINDOWS = (2, 4, 8, 16)
POOL_GC = POOL_WIDTH // len(POOL_WINDOWS)
DN_HEAD_DIM = 128
DN_WIDTH = MIX_WIDTH - POOL_WIDTH
DN_HEADS = DN_WIDTH // DN_HEAD_DIM
CONV_WIDTH = 5
CHUNK = 64
N_GROUPS = 4
EXPERTS_PER_GROUP = 8
N_EXPERTS = N_GROUPS * EXPERTS_PER_GROUP
TOP_K = 2
D_EXPERT = D_MODEL // 2
MOE_BLOCK = 128
EPS = 1e-6
Q0 = POOL_WIDTH
Z0 = Q0 + 3 * DN_WIDTH
AB0 = Z0 + DN_WIDTH
IN_COLS = AB0 + 4 * DN_HEADS

kernel_name = 'hybrid_pool_gdn_hmoe_dit_layer'


def _rmsnorm(x, g):
    xf = x.astype(jnp.float32)
    y = xf * lax.rsqrt(jnp.mean(xf * xf, axis=-1, keepdims=True) + EPS)
    return y.astype(x.dtype) * g


def _modulate(h, shift, scale):
    return h * (1 + scale) + shift


def _box_mean(u, window, rows, cols):
    b, t, ch = u.shape
    uf = u.astype(jnp.float32).reshape(b, rows, cols, ch)
    sat = jnp.pad(jnp.cumsum(jnp.cumsum(uf, axis=1), axis=2), ((0, 0), (1, 0), (1, 0), (0, 0)))
    lo = window // 2
    hi = window - lo
    def bounds(n):
        i = jnp.arange(n)
        return jnp.clip(i - lo, 0, n), jnp.clip(i + hi, 0, n)
    r0, r1 = bounds(rows)
    c0, c1 = bounds(cols)
    corner = lambda ri, ci: jnp.take(jnp.take(sat, ri, axis=1), ci, axis=2)
    total = corner(r1, c1) - corner(r0, c1) - corner(r1, c0) + corner(r0, c0)
    count = ((r1 - r0)[:, None] * (c1 - c0)[None, :]).astype(jnp.float32)
    return (total / count[None, :, :, None]).reshape(b, t, ch).astype(u.dtype)


def _pool_mixer(u, pool_w, pool_scale, rows, cols):
    outs = []
    for gi, win in enumerate(POOL_WINDOWS):
        ug = u[..., gi * POOL_GC:(gi + 1) * POOL_GC]
        outs.append((_box_mean(ug, win, rows, cols) - ug) @ pool_w[gi])
    return jnp.concatenate(outs, axis=-1) * pool_scale


def _short_conv(x, w):
    y = lax.conv_general_dilated(x, w[:, None, :], window_strides=(1,),
                                 padding=((CONV_WIDTH // 2, CONV_WIDTH // 2),),
                                 dimension_numbers=('NWC', 'WIO', 'NWC'),
                                 feature_group_count=x.shape[-1])
    return jax.nn.silu(y)


def _heads(a):
    b, t, _ = a.shape
    return a.reshape(b, t, DN_HEADS, DN_HEAD_DIM).transpose(0, 2, 1, 3)


def _l2norm(a):
    return a * lax.rsqrt(jnp.sum(a * a, axis=-1, keepdims=True) + EPS)


def _delta_inputs(proj, conv_w, a_log_f, dt_bias_f, a_log_b, dt_bias_b):
    qkv = _short_conv(proj[..., Q0:Z0], conv_w).astype(jnp.float32)
    q = _l2norm(_heads(qkv[..., :DN_WIDTH])) * (DN_HEAD_DIM ** -0.5)
    k = _l2norm(_heads(qkv[..., DN_WIDTH:2 * DN_WIDTH]))
    v = _heads(qkv[..., 2 * DN_WIDTH:])
    ab = proj[..., AB0:].astype(jnp.float32).transpose(0, 2, 1)
    h = DN_HEADS
    beta_f = jax.nn.sigmoid(ab[:, :h])
    beta_b = jax.nn.sigmoid(ab[:, h:2 * h])
    g_f = -jnp.exp(a_log_f.astype(jnp.float32))[:, None] * jax.nn.softplus(ab[:, 2 * h:3 * h] + dt_bias_f.astype(jnp.float32)[:, None])
    g_b = -jnp.exp(a_log_b.astype(jnp.float32))[:, None] * jax.nn.softplus(ab[:, 3 * h:] + dt_bias_b.astype(jnp.float32)[:, None])
    return q, k, v, (g_f, beta_f), (g_b, beta_b)


def _gated_delta_chunked(q, k, v, g, beta, s0):
    b, h, t, dk = q.shape
    dv = v.shape[-1]
    n = t // CHUNK
    split = lambda a: a.reshape(b, h, n, CHUNK, *a.shape[3:])
    q, k, v, g, beta = (split(a) for a in (q, k, v, g, beta))
    gc = jnp.cumsum(g, axis=-1)
    tri = jnp.tril(jnp.ones((CHUNK, CHUNK), dtype=bool))
    strict = tri & ~jnp.eye(CHUNK, dtype=bool)
    diff = gc[..., :, None] - gc[..., None, :]
    decay = jnp.where(tri, jnp.exp(jnp.where(tri, diff, 0.0)), 0.0)
    kb = k * beta[..., None]
    a_mat = jnp.where(strict, jnp.einsum('bhncd,bhnsd->bhncs', kb, k) * decay, 0.0)
    eye = jnp.eye(CHUNK, dtype=jnp.float32)
    t_inv = lax.linalg.triangular_solve(a_mat + eye, jnp.broadcast_to(eye, a_mat.shape),
                                        left_side=True, lower=True, unit_diagonal=True)
    u = t_inv @ (v * beta[..., None])
    w = t_inv @ (kb * jnp.exp(gc)[..., None])
    qk = jnp.einsum('bhncd,bhnsd->bhncs', q, k) * decay
    q_dec = q * jnp.exp(gc)[..., None]
    k_dec = k * jnp.exp(gc[..., -1:] - gc)[..., None]
    g_tot = jnp.exp(gc[..., -1])

    def step(s, xs):
        u_c, w_c, qk_c, qd_c, kd_c, gt_c = xs
        v_new = u_c - w_c @ s
        o_c = qd_c @ s + qk_c @ v_new
        s = s * gt_c[..., None, None] + jnp.einsum('bhcd,bhce->bhde', kd_c, v_new)
        return s, o_c

    xs = tuple(jnp.moveaxis(a, 2, 0) for a in (u, w, qk, q_dec, k_dec, g_tot))
    s, o = lax.scan(step, s0, xs)
    return jnp.moveaxis(o, 0, 2).reshape(b, h, t, dv), s


def _bidir_delta(lat, ctx):
    ql, kl, vl, fl, bl = lat
    qc, kc, vc, fc, bc = ctx
    b = ql.shape[0]
    s0 = jnp.zeros((b, DN_HEADS, DN_HEAD_DIM, DN_HEAD_DIM), jnp.float32)
    oc_f, s_f = _gated_delta_chunked(qc, kc, vc, fc[0], fc[1], s0)
    ol_f, _ = _gated_delta_chunked(ql, kl, vl, fl[0], fl[1], s_f)
    flip = lambda a: jnp.flip(a, axis=2)
    oc_b, s_b = _gated_delta_chunked(flip(qc), flip(kc), flip(vc), flip(bc[0]), flip(bc[1]), s0)
    ol_b, _ = _gated_delta_chunked(flip(ql), flip(kl), flip(vl), flip(bl[0]), flip(bl[1]), s_b)
    return ol_f + flip(ol_b), oc_f + flip(oc_b)


def _gated_out(o, z, out_norm_g):
    b, h, t, d = o.shape
    o = o.transpose(0, 2, 1, 3)
    o = o * lax.rsqrt(jnp.mean(o * o, axis=-1, keepdims=True) + EPS) * out_norm_g.astype(jnp.float32)
    return (o.reshape(b, t, h * d) * jax.nn.silu(z.astype(jnp.float32))).astype(z.dtype)


def _mix_stream(proj, o, pool_w, pool_scale, out_norm_g, w_out, rows, cols):
    pool = _pool_mixer(proj[..., :POOL_WIDTH], pool_w, pool_scale, rows, cols)
    dn = _gated_out(o, proj[..., Z0:AB0], out_norm_g)
    return jnp.concatenate([pool, dn], axis=-1) @ w_out


def _hier_moe(h, w_grp, b_grp, w_rt, b_rt, w1, w3, w2):
    n, d = h.shape
    hf = h.astype(jnp.float32)
    grp_logits = hf @ w_grp.astype(jnp.float32) + b_grp.astype(jnp.float32)
    g_sel = jnp.argmax(grp_logits, axis=-1)
    p_grp = jnp.take_along_axis(jax.nn.softmax(grp_logits, axis=-1), g_sel[:, None], axis=-1)
    e_logits = (hf @ w_rt.astype(jnp.float32) + b_rt.astype(jnp.float32)).reshape(n, N_GROUPS, EXPERTS_PER_GROUP)
    in_grp = jnp.take_along_axis(e_logits, g_sel[:, None, None], axis=1)[:, 0]
    top_val, top_loc = lax.top_k(in_grp, TOP_K)
    gate = jax.nn.softmax(top_val, axis=-1) * p_grp
    expert = (g_sel[:, None] * EXPERTS_PER_GROUP + top_loc).reshape(-1)
    a_cnt = n * TOP_K
    onehot = jax.nn.one_hot(expert, N_EXPERTS, dtype=jnp.int32)
    rank = jnp.sum((jnp.cumsum(onehot, axis=0) - onehot) * onehot, axis=-1)
    counts = jnp.sum(onehot, axis=0)
    padded = ((counts + MOE_BLOCK - 1) // MOE_BLOCK) * MOE_BLOCK
    ends = jnp.cumsum(padded)
    dest = (ends - padded)[expert] + rank
    n_blocks = (a_cnt + N_EXPERTS * (MOE_BLOCK - 1) + MOE_BLOCK - 1) // MOE_BLOCK
    tok = jnp.arange(a_cnt) // TOP_K
    x_pad = jnp.zeros((n_blocks * MOE_BLOCK, d), h.dtype).at[dest].set(h[tok])
    block_expert = jnp.minimum(jnp.searchsorted(ends, jnp.arange(n_blocks) * MOE_BLOCK, side='right'), N_EXPERTS - 1)

    def run_block(args):
        xb, e = args
        return (jax.nn.silu(xb @ w1[e]) * (xb @ w3[e])) @ w2[e]

    y_pad = lax.map(run_block, (x_pad.reshape(n_blocks, MOE_BLOCK, d), block_expert))
    y = y_pad.reshape(n_blocks * MOE_BLOCK, d)[dest].reshape(n, TOP_K, d)
    return jnp.einsum('nk,nkd->nd', gate.astype(h.dtype), y)


def setup_inputs(seed: int = 0) -> dict:
    key = jax.random.key(seed)
    ks = iter(jax.random.split(key, 40))
    nrm = lambda shape, scale: jax.random.normal(next(ks), shape, jnp.float32) * scale
    L, D, H = DEPTH, D_MODEL, DN_HEADS

    def dt_bias():
        dt = jnp.exp(jax.random.uniform(next(ks), (L, H), jnp.float32, math.log(1e-3), math.log(1e-1)))
        return dt + jnp.log(-jnp.expm1(-dt))

    def a_log():
        return jnp.log(jax.random.uniform(next(ks), (L, H), jnp.float32, 1.0, 16.0))

    return {
        'x': nrm((BATCH, SEQ, D), 1.0),
        'c': nrm((BATCH, D), 1.0),
        'ctx': nrm((BATCH, CTX_LEN, D), 1.0),
        'c_ctx': nrm((D,), 1.0),
        'w_mod': nrm((L, D, 6 * D), 0.5 * D ** -0.5),
        'b_mod': nrm((L, 6 * D), 0.02),
        'norm1_g': 1.0 + nrm((L, D), 0.05),
        'w_in': nrm((L, D, IN_COLS), D ** -0.5),
        'pool_w': nrm((L, len(POOL_WINDOWS), POOL_GC, POOL_GC), POOL_GC ** -0.5),
        'pool_scale': 1.0 + nrm((L, POOL_WIDTH), 0.1),
        'conv_w': nrm((L, CONV_WIDTH, 3 * DN_WIDTH), CONV_WIDTH ** -0.5),
        'a_log_f': a_log(),
        'dt_bias_f': dt_bias(),
        'a_log_b': a_log(),
        'dt_bias_b': dt_bias(),
        'out_norm_g': 1.0 + nrm((L, DN_HEAD_DIM), 0.05),
        'w_out': nrm((L, MIX_WIDTH, D), MIX_WIDTH ** -0.5),
        'norm2_g': 1.0 + nrm((L, D), 0.05),
        'w_grp': nrm((L, D, N_GROUPS), D ** -0.5),
        'b_grp': nrm((L, N_GROUPS), 0.01),
        'w_rt': nrm((L, D, N_EXPERTS), D ** -0.5),
        'b_rt': nrm((L, N_EXPERTS), 0.01),
        'w1': nrm((L, N_EXPERTS, D, D_EXPERT), D ** -0.5),
        'w3': nrm((L, N_EXPERTS, D, D_EXPERT), D ** -0.5),
        'w2': nrm((L, N_EXPERTS, D_EXPERT, D), D_EXPERT ** -0.5),
        'final_g': 1.0 + nrm((D,), 0.05),
    }


def reference(x, c, ctx, c_ctx, w_mod, b_mod, norm1_g, w_in, pool_w, pool_scale, conv_w,
              a_log_f, dt_bias_f, a_log_b, dt_bias_b, out_norm_g, w_out, norm2_g,
              w_grp, b_grp, w_rt, b_rt, w1, w3, w2, final_g):
    rows = x.shape[1] // GRID_W
    d = x.shape[-1]
    for l in range(DEPTH):
        last = l == DEPTH - 1
        mod = jax.nn.silu(c) @ w_mod[l] + b_mod[l]
        mod_c = jax.nn.silu(c_ctx) @ w_mod[l] + b_mod[l]
        sh1, sc1, gt1, sh2, sc2, gt2 = jnp.split(mod[:, None, :], 6, axis=-1)
        sh1c, sc1c, gt1c, sh2c, sc2c, gt2c = jnp.split(mod_c[None, None, :], 6, axis=-1)
        proj = _modulate(_rmsnorm(x, norm1_g[l]), sh1, sc1) @ w_in[l]
        proj_c = _modulate(_rmsnorm(ctx, norm1_g[l]), sh1c, sc1c) @ w_in[l]
        lat_in = _delta_inputs(proj, conv_w[l], a_log_f[l], dt_bias_f[l], a_log_b[l], dt_bias_b[l])
        ctx_in = _delta_inputs(proj_c, conv_w[l], a_log_f[l], dt_bias_f[l], a_log_b[l], dt_bias_b[l])
        o_lat, o_ctx = _bidir_delta(lat_in, ctx_in)
        x = x + gt1 * _mix_stream(proj, o_lat, pool_w[l], pool_scale[l], out_norm_g[l], w_out[l], rows, GRID_W)
        h2 = _modulate(_rmsnorm(x, norm2_g[l]), sh2, sc2)
        x = x + gt2 * _hier_moe(h2.reshape(-1, d), w_grp[l], b_grp[l], w_rt[l], b_rt[l], w1[l], w3[l], w2[l]).reshape(x.shape)
        if not last:
            ctx = ctx + gt1c * _mix_stream(proj_c, o_ctx, pool_w[l], pool_scale[l], out_norm_g[l], w_out[l], 1, ctx.shape[1])
            h2c = _modulate(_rmsnorm(ctx, norm2_g[l]), sh2c, sc2c)
            ctx = ctx + gt2c * _hier_moe(h2c.reshape(-1, d), w_grp[l], b_grp[l], w_rt[l], b_rt[l], w1[l], w3[l], w2[l]).reshape(ctx.shape)
    return _rmsnorm(x, final_g)
```

```python
import contextlib
import numpy as np
import concourse.bass as bass
import concourse.mybir as mybir
from concourse.bass_utils import run_bass_kernel_spmd

F32 = mybir.dt.float32
BF16 = mybir.dt.bfloat16
ALU = mybir.AluOpType
AF = mybir.ActivationFunctionType
AX = mybir.AxisListType

D = 2048
T = 2048
TC = 256
OWN = 1024
NE = 32
EPS = 1e-6
CAP = 128


class Res:
    __slots__ = ("name", "lw", "rd", "psum")

    def __init__(self, name="", psum=False):
        self.name = name
        self.lw = None
        self.rd = []
        self.psum = psum


class Op:
    __slots__ = ("eng", "fn", "waits", "signal", "seq", "dma", "dsem", "dval")

    def __init__(self, eng, fn, dma):
        self.eng = eng
        self.fn = fn
        self.waits = []
        self.signal = False
        self.seq = 0
        self.dma = dma
        self.dsem = None
        self.dval = 0


class Prog:
    def __init__(self, nc, n_dma_sems=16):
        self.nc = nc
        self.ops = []
        self.n_dma_sems = n_dma_sems
        self.dma_rr = 0
        self.dma_cnt = [0] * n_dma_sems
        self.bar = None
        self.last = {}
        self.last_dma = {}

    def barrier(self, fn):
        o = Op("pool", fn, False)
        for e, d in self.last.items():
            if e != "pool":
                o.waits.append(d)
                d.signal = True
        for sidx, d in self.last_dma.items():
            o.waits.append(d)
        o.signal = True
        self.ops.append(o)
        self.last["pool"] = o
        self.bar = o
        return o

    def _add(self, op, reads, writes):
        for r in list(reads):
            if r.psum:
                writes.append(r)
        if op.dma:
            self.last_dma[op.dsem] = op
        else:
            self.last[op.eng] = op
        if self.bar is not None and (op.dma or op.eng != "pool"):
            op.waits.append(self.bar)
        deps = []
        for r in reads:
            if r.lw is not None:
                deps.append((r.lw, True))
        for w in writes:
            if w.lw is not None:
                deps.append((w.lw, False))
            for o in w.rd:
                deps.append((o, False))
        for (d, raw) in deps:
            if d is op:
                continue
            if d.dma:
                op.waits.append(d)
                d.signal = True
            elif d.eng == op.eng and not op.dma:
                if raw and op.eng != "pe":
                    op.waits.append(d)
                    d.signal = True
            else:
                op.waits.append(d)
                d.signal = True
        for r in reads:
            r.rd.append(op)
        for w in writes:
            w.lw = op
            w.rd = []
        self.ops.append(op)
        return op

    def op(self, eng, fn, reads=(), writes=()):
        return self._add(Op(eng, fn, False), list(reads), list(writes))

    def dma(self, eng, fn, reads=(), writes=()):
        o = Op(eng, fn, True)
        s = self.dma_rr
        self.dma_rr = (self.dma_rr + 1) % self.n_dma_sems
        self.dma_cnt[s] += 16
        o.dsem = s
        o.dval = self.dma_cnt[s]
        prev = self.last_dma.get(s)
        self._add(o, list(reads), list(writes))
        if prev is not None:
            o.waits.append(prev)
        return o

    def emit(self, final_wait_ops=()):
        nc = self.nc
        engs = {"pe": nc.tensor, "act": nc.scalar, "dve": nc.vector, "pool": nc.gpsimd, "sp": nc.sync}
        with contextlib.ExitStack() as st:
            csem = {e: st.enter_context(nc.semaphore("c_" + e)) for e in engs}
            dsem = [st.enter_context(nc.semaphore("d_%d" % i)) for i in range(self.n_dma_sems)]
            cnt = {e: 0 for e in csem}
            for o in self.ops:
                if o.signal and not o.dma:
                    cnt[o.eng] += 1
                    o.seq = cnt[o.eng]
            seen = {e: {} for e in engs}
            nwait = 0
            for o in self.ops:
                E = engs[o.eng]
                need = {}
                for d in o.waits:
                    key = ("d", d.dsem) if d.dma else ("c", d.eng)
                    val = d.dval if d.dma else d.seq
                    if need.get(key, 0) < val:
                        need[key] = val
                for key, val in need.items():
                    if seen[o.eng].get(key, 0) >= val:
                        continue
                    seen[o.eng][key] = val
                    sem = dsem[key[1]] if key[0] == "d" else csem[key[1]]
                    E.wait_ge(sem, val)
                    nwait += 1
                ins = o.fn()
                if o.dma:
                    ins.then_inc(dsem[o.dsem], 16)
                elif o.signal:
                    ins.then_inc(csem[o.eng], 1)
            need = {}
            for d in final_wait_ops:
                key = ("d", d.dsem) if d.dma else ("c", d.eng)
                val = d.dval if d.dma else d.seq
                need[key] = max(need.get(key, 0), val)
            for key, val in need.items():
                sem = dsem[key[1]] if key[0] == "d" else csem[key[1]]
                nc.sync.wait_ge(sem, val)
            self.stats = dict(n_ops=len(self.ops), n_wait=nwait, cnt=cnt, dma=list(self.dma_cnt))


class _Stop(Exception):
    pass


class Tl:
    def __init__(self, t, n=1, psum=False):
        self.t = t
        self.r = [Res(psum=psum) for _ in range(n)]

    def __getitem__(self, idx):
        return self.t[idx]


def build(dbg=None, stop=None):
    nc = bass.Bass("TRN2", target_bir_lowering=False)
    P = Prog(nc)

    def din(name, shape):
        return nc.dram_tensor(name, list(shape), F32, kind="ExternalInput").ap()

    x_d = din("x", [T, D]); ctx_d = din("ctx", [TC, D]); cc_d = din("cc", [128, 16, 2])
    wmod_d = din("w_mod", [D, 6 * D]); bmod_d = din("bmodT", [128, 96]); g1_d = din("g1T", [128, 16])
    win_d = din("w_in", [D, 5152]); cw_d = din("cw", [128, 8, 3, 5])
    alog_d = din("alog_rep", [128, 18, 16]); dtb_d = din("dtb_rep", [128, 18, 16])
    ong_d = din("ong", [128, 1]); poolw_d = din("pool_w", [4, 256, 256]); ps_d = din("psT", [128, 8])
    pinv_d = din("pinv", [128, 4, OWN]); flip_d = din("flipc", [128, 2])
    wout_d = din("w_out", [D, D]); g2_d = din("g2_bc", [128, D]); fg_d = din("fg_bc", [128, D])
    wr_d = din("wr", [D, 36]); brow_d = din("brow", [128, 36])
    w1_d = din("w1", [NE, D, 1024]); w3_d = din("w3", [NE, D, 1024]); w2_d = din("w2", [NE, 1024, D])
    out_d = nc.dram_tensor("out", [OWN, D], F32, kind="ExternalOutput").ap()
    dbg_d = None
    if dbg:
        dbg_d = nc.dram_tensor("dbg", [128, dbg], F32, kind="ExternalOutput").ap()
    dbg_off = [0]
    out_dram = Res("out")
    dbg_res = Res("dbg")

    E = {"pe": nc.tensor, "act": nc.scalar, "dve": nc.vector, "pool": nc.gpsimd}

    def rs(tls):
        out = []
        for t in tls:
            if isinstance(t, Tl):
                out.extend(t.r)
            elif isinstance(t, Res):
                out.append(t)
            else:
                out.extend(t)
        return out

    def OP(eng, name, *args, r=(), w=(), **kw):
        return P.op(eng, lambda: getattr(E[eng], name)(*args, **kw), rs(r), rs(w))

    def MM(out, lhsT, rhs, start=True, stop=True, r=(), w=()):
        return P.op("pe", lambda: nc.tensor.matmul(out, lhsT, rhs, start=start, stop=stop), rs(r), rs(w))

    def TR(out, in_, ident_ap, r=(), w=()):
        return P.op("pe", lambda: nc.tensor.transpose(out, in_, ident_ap), rs(r), rs(w))

    def DMA(q, out, in_, r=(), w=()):
        eng = {"sp": nc.sync, "pool": nc.gpsimd, "act": nc.scalar}[q]
        return P.dma(q, lambda: eng.dma_start(out=out, in_=in_), rs(r), rs(w))

    final_ops = []

    def DBG(tl, ap, ncols):
        if not dbg:
            return
        o = dbg_off[0]
        dbg_off[0] += ncols
        assert dbg_off[0] <= dbg
        final_ops.append(DMA("sp", dbg_d[:, o:o + ncols], ap, r=[tl], w=[dbg_res]))

    try:
     with contextlib.ExitStack() as top:
        def sb(st, name, shape, dt=F32, n=1):
            return Tl(st.enter_context(nc.sbuf_tensor("s_" + name, list(shape), dt)), n)

        PS = [Tl(top.enter_context(nc.psum_tensor("ps%d" % i, [128, 512], F32)), psum=True) for i in range(8)]
        bscr = sb(top, "bscr", [128, 1])

        def BAR():
            P.barrier(lambda: nc.gpsimd.memset(bscr[:], 0.0))

        def CHK(k):
            if stop == k:
                raise _Stop()

        @contextlib.contextmanager
        def scope():
            BAR()
            with contextlib.ExitStack() as st_:
                yield st_
            BAR()

        ident = sb(top, "ident", [128, 128]); ones = sb(top, "ones", [128, 128])
        mL = sb(top, "mL", [128, 128]); mU = sb(top, "mU", [128, 128])
        mLs = sb(top, "mLs", [128, 128]); mUs = sb(top, "mUs", [128, 128])
        iota = sb(top, "iota", [128, 128])
        OP("pool", "memset", ones[:], 1.0, w=[ones])
        OP("pool", "memset", ident[:], 0.0, w=[ident])
        OP("pool", "affine_select", ident[:], ident[:], pattern=[[-1, 128]], compare_op=ALU.not_equal, fill=1.0,
           base=0, channel_multiplier=1, r=[ident], w=[ident])
        OP("pool", "affine_select", mL[:], ones[:], pattern=[[-1, 128]], compare_op=ALU.is_ge, fill=0.0,
           base=0, channel_multiplier=1, r=[ones], w=[mL])
        OP("pool", "affine_select", mLs[:], ones[:], pattern=[[-1, 128]], compare_op=ALU.is_gt, fill=0.0,
           base=0, channel_multiplier=1, r=[ones], w=[mLs])
        OP("pool", "affine_select", mU[:], ones[:], pattern=[[1, 128]], compare_op=ALU.is_ge, fill=0.0,
           base=0, channel_multiplier=-1, r=[ones], w=[mU])
        OP("pool", "affine_select", mUs[:], ones[:], pattern=[[1, 128]], compare_op=ALU.is_gt, fill=0.0,
           base=0, channel_multiplier=-1, r=[ones], w=[mUs])
        OP("pool", "tensor_scalar", mLs[:], mLs[:], -1.0, None, op0=ALU.mult, r=[mLs], w=[mLs])
        OP("pool", "tensor_scalar", mUs[:], mUs[:], -1.0, None, op0=ALU.mult, r=[mUs], w=[mUs])
        OP("pool", "iota", iota[:], pattern=[[1, 128]], base=0, channel_multiplier=0,
           allow_small_or_imprecise_dtypes=True, w=[iota])

        modT = sb(top, "modT", [128, 96, 2])
        catT = sb(top, "catT", [128, 16, OWN], BF16, n=16)
        small = sb(top, "small", [128, 64])
        ong = sb(top, "ong", [128, 1]); flipc = sb(top, "flipc", [128, 2]); psT = sb(top, "psT", [128, 8])
        DMA("sp", ong[:], ong_d, w=[ong]); DMA("sp", flipc[:], flip_d, w=[flipc]); DMA("sp", psT[:], ps_d, w=[psT])

        DBG(modT, modT[:].rearrange("p j t -> p (j t)"), 192)
        CHK(1)

        def modc(j, t):
            return modT[:, j, t:t + 1]

        with scope() as mx:
            h1T = sb(mx, "h1T", [128, 16, T + TC], BF16, n=18)
            a1 = sb(mx, "a1", [128, 16, 2]); g1T = sb(mx, "g1T", [128, 16]); sh1c = sb(mx, "sh1c", [128, 16, 2])
            DMA("sp", g1T[:], g1_d, w=[g1T])
            with scope() as st:
                cc = sb(st, "cc", [128, 16, 2]); scc = sb(st, "scc", [128, 16, 2]); bmod = sb(st, "bmod", [128, 96])
                wm = [sb(st, "wm%d" % i, [128, 16, 512]) for i in range(2)]
                xt = [sb(st, "xt%d" % i, [128, D]) for i in range(2)]
                junk = sb(st, "junk", [128, D], BF16)
                DMA("sp", cc[:], cc_d, w=[cc]); DMA("sp", bmod[:], bmod_d, w=[bmod])
                OP("act", "activation", scc[:], cc[:], AF.Silu, r=[cc], w=[scc])
                wmv = wmod_d.rearrange("(k p) n -> p k n", p=128)

                def mod_group(jg, psm):
                    wb = wm[jg % 2]
                    DMA("sp" if jg % 2 == 0 else "act", wb[:], wmv[:, :, jg * 512:(jg + 1) * 512], w=[wb])
                    for jc in range(4):
                        j = jg * 4 + jc
                        for k in range(16):
                            MM(psm[:, 2 * j:2 * j + 2], wb[:, k, jc * 128:(jc + 1) * 128], scc[:, k, :],
                               start=(k == 0), stop=(k == 15), r=[wb, scc], w=[psm])

                def mod_evac(psm, j0, j1):
                    pv = psm[:, 2 * j0:2 * j1].rearrange("p (j t) -> p j t", t=2)
                    for t in range(2):
                        OP("dve", "tensor_tensor", modT[:, j0:j1, t], pv[:, :, t], bmod[:, j0:j1], op=ALU.add, r=[psm, bmod], w=[modT])

                for jg in range(8):
                    mod_group(jg, PS[4])
                mod_evac(PS[4], 0, 32)
                for t in range(2):
                    OP("dve", "scalar_tensor_tensor", a1[:, :, t], modT[:, 16:32, t], 1.0, g1T[:], op0=ALU.add, op1=ALU.mult,
                       r=[modT, g1T], w=[a1])
                OP("dve", "tensor_copy", sh1c[:], modT[:, 0:16, :], r=[modT], w=[sh1c])
                for i in range(18):
                    xb = xt[i % 2]
                    src = x_d[i * 128:(i + 1) * 128, :] if i < 16 else ctx_d[(i - 16) * 128:(i - 15) * 128, :]
                    t = 0 if i < 16 else 1
                    DMA("sp", xb[:], src, w=[xb])
                    c0 = small[:, 0:1]; c1 = small[:, 1:2]; c2 = small[:, 2:3]
                    OP("act", "activation", junk[:], xb[:], AF.Square, accum_out=c0, r=[xb], w=[junk, small])
                    OP("act", "activation", c1, c0, AF.Sqrt, scale=1.0 / D, bias=EPS, r=[small], w=[small])
                    OP("dve", "reciprocal", c2, c1, r=[small], w=[small])
                    OP("dve", "tensor_scalar", xb[:], xb[:], c2, None, op0=ALU.mult, r=[xb, small], w=[xb])
                    for kg in range(4):
                        pb = PS[kg]
                        for kk in range(4):
                            k = kg * 4 + kk
                            TR(pb[:, kk * 128:(kk + 1) * 128], xb[:, k * 128:(k + 1) * 128], ident[:], r=[xb, ident], w=[pb])
                        for kk in range(4):
                            k = kg * 4 + kk
                            dst = h1T[:, k, i * 128:(i + 1) * 128]
                            if kk % 2 == 0:
                                OP("act", "activation", dst, pb[:, kk * 128:(kk + 1) * 128], AF.Identity,
                                   scale=a1[:, k, t:t + 1], bias=sh1c[:, k, t:t + 1], r=[pb, a1, sh1c], w=[h1T.r[i]])
                            else:
                                OP("dve", "tensor_scalar", dst, pb[:, kk * 128:(kk + 1) * 128], a1[:, k, t:t + 1], sh1c[:, k, t:t + 1],
                                   op0=ALU.mult, op1=ALU.add, r=[pb, a1, sh1c], w=[h1T.r[i]])
                    if i < 16:
                        mod_group(8 + i, PS[5])
                mod_evac(PS[5], 32, 96)
            CHK(2)
            winv = win_d.rearrange("(k p) n -> p k n", p=128)
            wpc = [sb(mx, "wpc%d" % i, [128, 16, 128], BF16) for i in range(1)]
            wp_i = [0]

            def load_w(col0, ncol=128):
                wb = wpc[0]
                wp_i[0] += 1
                DMA("pool", wb[:, :, 0:ncol], winv[:, :, col0:col0 + ncol], w=[wb])
                return wb

            def proj(wb, M, tok0, n, pbank):
                tiles = h1T.r[tok0 // 128:(tok0 + n + 127) // 128]
                for k in range(16):
                    MM(pbank[0:M, 0:n], wb[:, k, 0:M], h1T[:, k, tok0:tok0 + n], start=(k == 0), stop=(k == 15),
                       r=[wb] + tiles, w=[pbank])

            NTL = 18
            gates = {}
            for nm in ("beta", "gc", "ekd", "bg", "gtot"):
                gates[nm] = sb(mx, "g_" + nm, [128, NTL, 16])
            with scope() as st:
                abT = sb(st, "abT", [32, T + TC]); ab_tok = sb(st, "ab_tok", [128, NTL, 32])
                alog = sb(st, "alog", [128, NTL, 16]); dtb = sb(st, "dtb", [128, NTL, 16])
                gtk = sb(st, "gtk", [128, NTL, 16]); tmpg = sb(st, "tmpg", [128, NTL, 16]); tot = sb(st, "tot", [128, NTL, 16])
                DMA("sp", alog[:], alog_d, w=[alog]); DMA("sp", dtb[:], dtb_d, w=[dtb])
                wb = load_w(5120, 32)
                for nt in range(5):
                    n = 512 if nt < 4 else 256
                    pb = PS[nt % 4]
                    proj(wb, 32, nt * 512, n, pb)
                    OP("act", "copy", abT[0:32, nt * 512:nt * 512 + n], pb[0:32, 0:n], r=[pb], w=[abT])
                pb = PS[4]
                for i in range(NTL):
                    TR(pb[:, (i % 16) * 32:(i % 16) * 32 + 32], abT[0:32, i * 128:(i + 1) * 128], ident[0:32, 0:32], r=[abT, ident], w=[pb])
                    if i == 15:
                        OP("act", "copy", ab_tok[:, 0:16, :], pb[:, 0:512].rearrange("p (i c) -> p i c", c=32), r=[pb], w=[ab_tok])
                OP("act", "copy", ab_tok[:, 16:18, :], pb[:, 0:64].rearrange("p (i c) -> p i c", c=32), r=[pb], w=[ab_tok])
                OP("act", "activation", gates["beta"][:], ab_tok[:, :, 0:16], AF.Sigmoid, r=[ab_tok], w=[gates["beta"]])
                OP("dve", "tensor_tensor", tmpg[:], ab_tok[:, :, 16:32], dtb[:], op=ALU.add, r=[ab_tok, dtb], w=[tmpg])
                OP("act", "activation", tmpg[:], tmpg[:], AF.Exp, r=[tmpg], w=[tmpg])
                OP("act", "activation", tmpg[:], tmpg[:], AF.Ln, bias=1.0, r=[tmpg], w=[tmpg])
                OP("act", "activation", alog[:], alog[:], AF.Exp, r=[alog], w=[alog])
                OP("dve", "scalar_tensor_tensor", gtk[:], tmpg[:], -1.0, alog[:], op0=ALU.mult, op1=ALU.mult, r=[tmpg, alog], w=[gtk])
                pg = PS[5]; pt = PS[6]
                for i in range(NTL):
                    MM(pg[:, i * 16:i * 16 + 8], mU[:], gtk[:, i, 0:8], r=[mU, gtk], w=[pg])
                    MM(pg[:, i * 16 + 8:i * 16 + 16], mL[:], gtk[:, i, 8:16], r=[mL, gtk], w=[pg])
                    MM(pt[:, i * 16:i * 16 + 16], ones[:], gtk[:, i, :], r=[ones, gtk], w=[pt])
                gc = gates["gc"]
                OP("act", "copy", gc[:], pg[:, 0:NTL * 16].rearrange("p (i c) -> p i c", c=16), r=[pg], w=[gc])
                OP("act", "copy", tot[:], pt[:, 0:NTL * 16].rearrange("p (i c) -> p i c", c=16), r=[pt], w=[tot])
                egc_t = sb(st, "egc_t", [128, NTL, 16])
                OP("act", "activation", egc_t[:], gc[:], AF.Exp, r=[gc], w=[egc_t])
                OP("act", "activation", gates["gtot"][:], tot[:], AF.Exp, r=[tot], w=[gates["gtot"]])
                OP("dve", "tensor_tensor", tmpg[:], tot[:], gc[:], op=ALU.subtract, r=[tot, gc, tmpg], w=[tmpg])
                OP("act", "activation", gates["ekd"][:], tmpg[:], AF.Exp, r=[tmpg], w=[gates["ekd"]])
                OP("dve", "tensor_tensor", gates["bg"][:], gates["beta"][:], egc_t[:], op=ALU.mult,
                   r=[gates["beta"], egc_t], w=[gates["bg"]])

            CHK(3)
            with scope() as hs:
                A = sb(hs, "A", [128, 2312]); qT = sb(hs, "qT", [128, OWN + TC]); kT = sb(hs, "kT", [128, T + TC])
                vT = sb(hs, "vT", [128, T + TC]); k_tok = sb(hs, "k_tok", [128, NTL, 128], n=NTL); v_tok = sb(hs, "v_tok", [128, NTL, 128], n=NTL)
                cw = sb(hs, "cw", [128, 8, 3, 5]); o_acc = sb(hs, "o_acc", [128, 8, 128], n=8)
                sq = sb(hs, "sq", [128, 256]); rt = sb(hs, "rt", [128, 256])
                NS = 2
                uw = [[sb(hs, "uw%d_%d" % (s, i), [128, 256]) for i in range(NS)] for s in range(2)]
                kdec = [[sb(hs, "kd%d_%d" % (s, i), [128, 128]) for i in range(NS)] for s in range(2)]
                qkT = [[sb(hs, "qk%d_%d" % (s, i), [128, 128]) for i in range(NS)] for s in range(2)]
                qdT = [[sb(hs, "qd%d_%d" % (s, i), [128, 128]) for i in range(NS)] for s in range(2)]
                Pb = [[sb(hs, "P%d_%d" % (s, i), [128, 128]) for i in range(2)] for s in range(2)]
                QR = [[sb(hs, "QR%d_%d" % (s, i), [128, 256]) for i in range(2)] for s in range(2)]
                gcb = [sb(hs, "gcb%d" % s, [128, 128]) for s in range(2)]
                tA = [sb(hs, "tA%d" % s, [128, 128]) for s in range(2)]; tB = [sb(hs, "tB%d" % s, [128, 128]) for s in range(2)]
                E1 = [sb(hs, "E1%d" % s, [128, 128]) for s in range(2)]; E2 = [sb(hs, "E2%d" % s, [128, 128]) for s in range(2)]
                Eg = tA
                GE = [sb(hs, "GE%d" % s, [128, 128]) for s in range(2)]
                TB = [sb(hs, "TB%d" % s, [128, 128]) for s in range(2)]; TBG = [sb(hs, "TBG%d" % s, [128, 128]) for s in range(2)]
                Sst = [[sb(hs, "S%d_%d" % (s, i), [128, 128]) for i in range(2)] for s in range(2)]
                vnew = [sb(hs, "vn%d" % s, [128, 128]) for s in range(2)]
                DMA("sp", cw[:], cw_d, w=[cw])
                OP("pool", "memset", A[:], 0.0, w=[A])

                def conv(dst, dcol, src0, n, h, j):
                    o = dst[:, dcol:dcol + n]
                    OP("dve", "tensor_scalar", o, A[:, src0:src0 + n], cw[:, h, j, 0:1], None, op0=ALU.mult, r=[A, cw], w=[dst])
                    for tap in range(1, 5):
                        OP("dve", "scalar_tensor_tensor", o, A[:, src0 + tap:src0 + tap + n], cw[:, h, j, tap:tap + 1], o,
                           op0=ALU.mult, op1=ALU.add, r=[A, cw, dst], w=[dst])
                    OP("act", "activation", o, o, AF.Silu, r=[dst], w=[dst])

                def l2norm(dst, col0, n, scale, bias):
                    if n > 256:
                        for o_ in range(0, n, 256):
                            l2norm(dst, col0 + o_, 256, scale, bias)
                        return
                    o = dst[:, col0:col0 + n]
                    pb = PS[7]
                    OP("act", "activation", sq[:, 0:n], o, AF.Square, r=[dst], w=[sq])
                    MM(pb[:, 0:n], ones[:], sq[:, 0:n], r=[ones, sq], w=[pb])
                    OP("act", "activation", rt[:, 0:n], pb[:, 0:n], AF.Sqrt, scale=scale, bias=bias, r=[pb], w=[rt])
                    OP("dve", "reciprocal", rt[:, 0:n], rt[:, 0:n], r=[rt], w=[rt])
                    OP("dve", "tensor_tensor", o, o, rt[:, 0:n], op=ALU.mult, r=[dst, rt], w=[dst])

                for h in range(8):
                    cbase = 1024 + h * 512
                    wb = load_w(cbase)
                    for (tok0, n, a0) in ((0, 512, 2), (512, 512, 514), (1024, 128, 1026), (T, 256, 2054)):
                        pb = PS[(tok0 // 512) % 4]
                        proj(wb, 128, tok0, n, pb)
                        OP("act", "copy", A[:, a0:a0 + n], pb[:, 0:n], r=[pb], w=[A])
                    conv(qT, 0, 0, OWN, h, 0)
                    conv(qT, OWN, 2052, TC, h, 0)
                    for c0 in (0, 512, 1024):
                        l2norm(qT, c0, 512 if c0 < 1024 else 256, 128.0, 128.0 * EPS)
                    wb = load_w(cbase + 128)
                    for nt in range(5):
                        n = 512 if nt < 4 else 256
                        a0 = 2 + nt * 512 if nt < 4 else 2054
                        pb = PS[nt % 4]
                        proj(wb, 128, nt * 512, n, pb)
                        OP("act", "copy", A[:, a0:a0 + n], pb[:, 0:n], r=[pb], w=[A])
                    conv(kT, 0, 0, T, h, 1)
                    conv(kT, T, 2052, TC, h, 1)
                    for nt in range(5):
                        l2norm(kT, nt * 512, 512 if nt < 4 else 256, 1.0, EPS)
                    for g in range(5):
                        pb = PS[g % 4]
                        ng = 4 if g < 4 else 2
                        for ii in range(ng):
                            i = g * 4 + ii
                            TR(pb[:, ii * 128:(ii + 1) * 128], kT[:, i * 128:(i + 1) * 128], ident[:], r=[kT, ident], w=[pb])
                        OP("act", "copy", k_tok[:, g * 4:g * 4 + ng, :], pb[:, 0:ng * 128].rearrange("p (i c) -> p i c", c=128),
                           r=[pb], w=k_tok.r[g * 4:g * 4 + ng])
                    wb = load_w(cbase + 256)
                    for nt in range(5):
                        n = 512 if nt < 4 else 256
                        a0 = 2 + nt * 512 if nt < 4 else 2054
                        pb = PS[nt % 4]
                        proj(wb, 128, nt * 512, n, pb)
                        OP("act", "copy", A[:, a0:a0 + n], pb[:, 0:n], r=[pb], w=[A])
                    conv(vT, 0, 0, T, h, 2)
                    conv(vT, T, 2052, TC, h, 2)
                    for g in range(5):
                        pb = PS[g % 4]
                        ng = 4 if g < 4 else 2
                        for ii in range(ng):
                            i = g * 4 + ii
                            TR(pb[:, ii * 128:(ii + 1) * 128], vT[:, i * 128:(i + 1) * 128], ident[:], r=[vT, ident], w=[pb])
                        OP("act", "copy", v_tok[:, g * 4:g * 4 + ng, :], pb[:, 0:ng * 128].rearrange("p (i c) -> p i c", c=128),
                           r=[pb], w=v_tok.r[g * 4:g * 4 + ng])
                    wb = load_w(cbase + 384)
                    for nt in range(2):
                        pb = PS[nt]
                        proj(wb, 128, nt * 512, 512, pb)
                        OP("act", "activation", catT[:, 8 + h, nt * 512:(nt + 1) * 512], pb[:, 0:512], AF.Silu, r=[pb], w=[catT.r[8 + h]])
                    if h == 0:
                        DBG(qT, qT[:, 0:256], 256); DBG(kT, kT[:, 0:256], 256); DBG(vT, vT[:, 0:256], 256)
                        DBG(k_tok, k_tok[:, 16, :], 128)

                    def prep(s, i, slot, want_out):
                        j = s * 8 + h
                        col = lambda nm: gates[nm][:, i, j:j + 1]
                        M1s = mLs if s == 0 else mUs
                        M2 = mU if s == 0 else mL
                        kc = kT[:, i * 128:(i + 1) * 128]
                        pA = PS[s]; pD = PS[2 + s]
                        OP("pool", "tensor_copy", gcb[s][:], col("gc").to_broadcast([128, 128]), r=[gates["gc"]], w=[gcb[s]])
                        MM(pA[:, 0:128], gcb[s][:], ident[:], r=[gcb[s], ident], w=[pA])
                        MM(pA[:, 128:256], kc, kc, r=[kT], w=[pA])
                        if want_out:
                            qc = qT[:, i * 128:(i + 1) * 128]
                            MM(pA[:, 256:384], kc, qc, r=[kT, qT], w=[pA])
                        yield
                        OP("dve", "tensor_scalar", tA[s][:], pA[:, 0:128], col("gc"), 0.0, op0=ALU.subtract, op1=ALU.max,
                           r=[pA, gates["gc"]], w=[tA[s]])
                        if want_out:
                            OP("dve", "tensor_scalar", tB[s][:], pA[:, 0:128], col("gc"), 0.0, op0=ALU.subtract, op1=ALU.min,
                               r=[pA, gates["gc"]], w=[tB[s]])
                        OP("act", "activation", E1[s][:], tA[s][:], AF.Exp, scale=-1.0, r=[tA[s]], w=[E1[s]])
                        if want_out:
                            OP("act", "activation", E2[s][:], tB[s][:], AF.Exp, r=[tB[s]], w=[E2[s]])
                            OP("act", "activation", Eg[s][:], pA[:, 0:128], AF.Exp, r=[pA], w=[Eg[s]])
                        yield
                        OP("dve", "tensor_tensor", GE[s][:], pA[:, 128:256], E1[s][:], op=ALU.mult, r=[pA, E1[s]], w=[GE[s]])
                        P0 = Pb[s][0]; QR0 = QR[s][0]
                        OP("dve", "scalar_tensor_tensor", P0[:], GE[s][:], col("beta"), M1s[:], op0=ALU.mult, op1=ALU.mult,
                           r=[GE[s], gates["beta"], M1s], w=[P0])
                        if want_out:
                            OP("pool", "tensor_tensor", E2[s][:], E2[s][:], M2[:], op=ALU.mult, r=[E2[s], M2], w=[E2[s]])
                            OP("dve", "tensor_tensor", qkT[s][slot][:], pA[:, 256:384], E2[s][:], op=ALU.mult, r=[pA, E2[s]], w=[qkT[s][slot]])
                            OP("pool", "tensor_tensor", qdT[s][slot][:], qT[:, i * 128:(i + 1) * 128], Eg[s][:], op=ALU.mult,
                               r=[qT, Eg[s]], w=[qdT[s][slot]])
                        yield
                        TR(pA[:, 384:512], P0[:], ident[:], r=[P0, ident], w=[pA])
                        yield
                        OP("act", "copy", QR0[:, 0:128], pA[:, 384:512], r=[pA], w=[QR0])
                        yield
                        pd = pD
                        MM(pd[:, 0:128], P0[:], QR0[:, 0:128], r=[P0, QR0], w=[pd])
                        MM(pd[:, 256:384], QR0[:, 0:128], P0[:], r=[P0, QR0], w=[pd])
                        yield
                        P1 = Pb[s][1]; QR1 = QR[s][1]
                        OP("act", "copy", QR1[:, 0:128], pd[:, 0:128], r=[pd], w=[QR1])
                        OP("dve", "tensor_copy", P1[:], pd[:, 256:384], r=[pd], w=[P1])
                        OP("pool", "tensor_tensor", QR1[:, 128:256], QR0[:, 0:128], ident[:], op=ALU.add, r=[QR0, ident], w=[QR1])
                        yield
                        cur = 1
                        for lv in range(1, 7):
                            Pc = Pb[s][cur]; QRc = QR[s][cur]; Pn = Pb[s][1 - cur]; QRn = QR[s][1 - cur]
                            if lv < 6:
                                MM(pd[:, 0:256], Pc[:], QRc[:, 0:256], r=[Pc, QRc], w=[pd])
                                MM(pd[:, 256:384], QRc[:, 0:128], Pc[:], r=[Pc, QRc], w=[pd])
                                yield
                                OP("act", "copy", QRn[:, 0:128], pd[:, 0:128], r=[pd], w=[QRn])
                                OP("dve", "tensor_tensor", QRn[:, 128:256], QRc[:, 128:256], pd[:, 128:256], op=ALU.add, r=[pd, QRc], w=[QRn])
                                OP("act", "copy", Pn[:], pd[:, 256:384], r=[pd], w=[Pn])
                                yield
                            else:
                                MM(pd[:, 128:256], Pc[:], QRc[:, 128:256], r=[Pc, QRc], w=[pd])
                                yield
                                OP("dve", "tensor_tensor", QRn[:, 128:256], QRc[:, 128:256], pd[:, 128:256], op=ALU.add, r=[pd, QRc], w=[QRn])
                                yield
                            cur = 1 - cur
                        Rf = QR[s][cur]
                        OP("dve", "tensor_scalar", TB[s][:], Rf[:, 128:256], col("beta"), None, op0=ALU.mult, r=[Rf, gates["beta"]], w=[TB[s]])
                        OP("pool", "tensor_scalar", TBG[s][:], Rf[:, 128:256], col("bg"), None, op0=ALU.mult, r=[Rf, gates["bg"]], w=[TBG[s]])
                        OP("pool", "tensor_scalar", kdec[s][slot][:], k_tok[:, i, :], col("ekd"), None, op0=ALU.mult,
                           r=[k_tok.r[i], gates["ekd"]], w=[kdec[s][slot]])
                        yield
                        MM(pd[:, 0:128], TB[s][:], v_tok[:, i, :], r=[TB[s], v_tok.r[i]], w=[pd])
                        MM(pd[:, 128:256], k_tok[:, i, :], TBG[s][:], r=[TBG[s], k_tok.r[i]], w=[pd])
                        yield
                        OP("act", "copy", uw[s][slot][:], pd[:, 0:256], r=[pd], w=[uw[s][slot]])
                        yield

                    def scan(s, i, slot, want_out, stp):
                        j = s * 8 + h
                        Sc = Sst[s][stp % 2]; Sn = Sst[s][(stp + 1) % 2]
                        pS = PS[4 + s]
                        MM(pS[:, 0:128], uw[s][slot][:, 128:256], Sc[:], r=[uw[s][slot], Sc], w=[pS])
                        yield
                        OP("dve", "tensor_tensor", vnew[s][:], uw[s][slot][:, 0:128], pS[:, 0:128], op=ALU.subtract,
                           r=[uw[s][slot], pS], w=[vnew[s]])
                        yield
                        if want_out:
                            MM(pS[:, 128:256], qdT[s][slot][:], Sc[:], start=True, stop=False, r=[qdT[s][slot], Sc], w=[pS])
                            MM(pS[:, 128:256], qkT[s][slot][:], vnew[s][:], start=False, stop=True, r=[qkT[s][slot], vnew[s]], w=[pS])
                        MM(pS[:, 256:384], kdec[s][slot][:], vnew[s][:], r=[kdec[s][slot], vnew[s]], w=[pS])
                        yield
                        if want_out:
                            if s == 0:
                                OP("act", "copy", o_acc[:, i, :], pS[:, 128:256], r=[pS], w=[o_acc.r[i]])
                            else:
                                OP("dve", "tensor_tensor", o_acc[:, i, :], o_acc[:, i, :], pS[:, 128:256], op=ALU.add,
                                   r=[pS, o_acc.r[i]], w=[o_acc.r[i]])
                        OP("dve", "scalar_tensor_tensor", Sn[:], Sc[:], gates["gtot"][:, i, j:j + 1], pS[:, 256:384],
                           op0=ALU.mult, op1=ALU.add, r=[Sc, gates["gtot"], pS], w=[Sn])
                        yield

                    def run_pair(gens):
                        gens = [g for g in gens if g is not None]
                        while gens:
                            for g in list(gens):
                                try:
                                    next(g)
                                except StopIteration:
                                    gens.remove(g)

                    seqF = [16, 17] + list(range(8))
                    seqB = [17, 16] + list(range(15, -1, -1))
                    for s in range(2):
                        OP("pool", "memset", Sst[s][0][:], 0.0, w=[Sst[s][0]])
                    def mk(kind, s_, n_):
                        seq = seqF if s_ == 0 else seqB
                        if n_ < 0 or n_ >= len(seq):
                            return None
                        i_ = seq[n_]
                        if kind == "prep":
                            return prep(s_, i_, n_ % NS, i_ < 8)
                        return scan(s_, i_, n_ % NS, i_ < 8, n_)

                    for n in range(19):
                        run_pair([mk("prep", 0, n), mk("prep", 1, n), mk("scan", 0, n - 1), mk("scan", 1, n - 1)])
                    if h == 0:
                        DBG(o_acc, o_acc[:, 0, :], 128); DBG(o_acc, o_acc[:, 7, :], 128)
                        CHK(4)
                    for i in range(8):
                        OP("act", "activation", sq[:, 0:128], o_acc[:, i, :], AF.Square, accum_out=small[:, 8 + i:9 + i],
                           r=[o_acc.r[i]], w=[sq, small])
                    OP("act", "activation", small[:, 16:24], small[:, 8:16], AF.Sqrt, scale=1.0 / 128, bias=EPS, r=[small], w=[small])
                    OP("dve", "reciprocal", small[:, 24:32], small[:, 16:24], r=[small], w=[small])
                    for i in range(8):
                        OP("dve", "tensor_scalar", o_acc[:, i, :], o_acc[:, i, :], small[:, 24 + i:25 + i], None, op0=ALU.mult,
                           r=[o_acc.r[i], small], w=[o_acc.r[i]])
                    for g in range(2):
                        pb = PS[g]
                        for ii in range(4):
                            i = g * 4 + ii
                            TR(pb[:, ii * 128:(ii + 1) * 128], o_acc[:, i, :], ident[:], r=[o_acc.r[i], ident], w=[pb])
                        OP("dve", "scalar_tensor_tensor", catT[:, 8 + h, g * 512:(g + 1) * 512], pb[:, 0:512], ong[:, 0:1],
                           catT[:, 8 + h, g * 512:(g + 1) * 512], op0=ALU.mult, op1=ALU.mult, r=[pb, ong, catT.r[8 + h]], w=[catT.r[8 + h]])

            CHK(5)
            with scope() as st:
                RW, CW = 32, 80
                NPF = RW * CW
                U = sb(st, "U", [128, NPF]); Ba = sb(st, "Ba", [128, NPF]); Bb = sb(st, "Bb", [128, NPF]); Bc = sb(st, "Bc", [128, NPF])
                pinv = sb(st, "pinv", [128, 4, OWN]); dT = [sb(st, "dT%d" % i, [128, OWN], BF16) for i in range(2)]
                pw = sb(st, "pw", [128, 2, 256], BF16)
                DMA("sp", pinv[:], pinv_d, w=[pinv])
                OP("pool", "memset", U[:], 0.0, w=[U])
                fa = flipc[:, 0:1]; fb = flipc[:, 1:2]

                def v3(tl):
                    return tl[:, 0:NPF].rearrange("p (r c) -> p r c", c=CW)

                def lvl1_cols(src, tmp, dst):
                    s3, t3, d3 = v3(src), v3(tmp), v3(dst)
                    OP("dve", "scalar_tensor_tensor", t3[:, :, 1:CW], s3[:, :, 0:CW - 1], fa, s3[:, :, 1:CW], op0=ALU.mult, op1=ALU.add,
                       r=[src, flipc], w=[tmp])
                    OP("pool", "tensor_copy", t3[:, :, 0:1], s3[:, :, 0:1], r=[src], w=[tmp])
                    OP("dve", "scalar_tensor_tensor", d3[:, :, 0:CW - 1], s3[:, :, 1:CW], fb, t3[:, :, 0:CW - 1], op0=ALU.mult, op1=ALU.add,
                       r=[src, tmp, flipc], w=[dst])
                    OP("pool", "tensor_copy", d3[:, :, CW - 1:CW], t3[:, :, CW - 1:CW], r=[tmp], w=[dst])

                def lvl_cols(src, dst, d):
                    s3, d3 = v3(src), v3(dst)
                    OP("dve", "tensor_tensor", d3[:, :, d:CW - d], s3[:, :, 2 * d:CW], s3[:, :, 0:CW - 2 * d], op=ALU.add, r=[src], w=[dst])
                    OP("pool", "tensor_copy", d3[:, :, 0:d], s3[:, :, d:2 * d], r=[src], w=[dst])
                    OP("pool", "tensor_copy", d3[:, :, CW - d:CW], s3[:, :, CW - 2 * d:CW - d], r=[src], w=[dst])

                def lvl1_rows(src, tmp, dst):
                    OP("dve", "scalar_tensor_tensor", tmp[:, CW:NPF], src[:, 0:NPF - CW], fa, src[:, CW:NPF], op0=ALU.mult, op1=ALU.add,
                       r=[src, flipc], w=[tmp])
                    OP("pool", "tensor_copy", tmp[:, 0:CW], src[:, 0:CW], r=[src], w=[tmp])
                    OP("dve", "scalar_tensor_tensor", dst[:, 0:NPF - CW], src[:, CW:NPF], fb, tmp[:, 0:NPF - CW], op0=ALU.mult, op1=ALU.add,
                       r=[src, tmp, flipc], w=[dst])
                    OP("pool", "tensor_copy", dst[:, NPF - CW:NPF], tmp[:, NPF - CW:NPF], r=[tmp], w=[dst])

                def lvl_rows(src, dst, d):
                    e = CW * d
                    OP("dve", "tensor_tensor", dst[:, e:NPF - e], src[:, 2 * e:NPF], src[:, 0:NPF - 2 * e], op=ALU.add, r=[src], w=[dst])
                    OP("pool", "tensor_copy", dst[:, 0:e], src[:, e:2 * e], r=[src], w=[dst])
                    OP("pool", "tensor_copy", dst[:, NPF - e:NPF], src[:, NPF - 2 * e:NPF - e], r=[src], w=[dst])

                for ci in range(8):
                    g = ci // 2
                    wb = load_w(ci * 128)
                    for nt in range(3):
                        pb = PS[nt]
                        proj(wb, 128, nt * 512, 512, pb)
                        OP("act", "copy", v3(U)[:, 8 + nt * 8:16 + nt * 8, 8:72], pb[:, 0:512].rearrange("p (r c) -> p r c", c=64), r=[pb], w=[U])
                    lvl1_cols(U, Ba, Bb)
                    cur, oth = Bb, Ba
                    for lv in range(1, g + 1):
                        lvl_cols(cur, oth, 2 ** (lv - 1))
                        cur, oth = oth, cur
                    lvl1_rows(cur, oth, Bc)
                    cur, oth = Bc, oth
                    for lv in range(1, g + 1):
                        lvl_rows(cur, oth, 2 ** (lv - 1))
                        cur, oth = oth, cur
                    dd = dT[ci % 2]
                    ci3 = v3(cur)[:, 8:24, 8:72]
                    OP("dve", "tensor_tensor", ci3, ci3, pinv[:, g, :].rearrange("p (r c) -> p r c", c=64), op=ALU.mult, r=[cur, pinv], w=[cur])
                    OP("dve", "tensor_tensor", dd[:].rearrange("p (r c) -> p r c", c=64), ci3, v3(U)[:, 8:24, 8:72], op=ALU.subtract, r=[cur, U], w=[dd])
                    if ci % 2 == 1:
                        DMA("pool", pw[:], poolw_d[g].rearrange("(k p) n -> p k n", p=128), w=[pw])
                        for co in range(2):
                            for nt in range(2):
                                pb = PS[4 + nt]
                                for k in range(2):
                                    MM(pb[:, 0:512], pw[:, k, co * 128:(co + 1) * 128], dT[k][:, nt * 512:(nt + 1) * 512],
                                       start=(k == 0), stop=(k == 1), r=[pw, dT[k]], w=[pb])
                                OP("act", "activation", catT[:, 2 * g + co, nt * 512:(nt + 1) * 512], pb[:, 0:512], AF.Identity,
                                   scale=psT[:, 2 * g + co:2 * g + co + 1], r=[pb, psT], w=[catT.r[2 * g + co]])

        with scope() as pm:
            xacc = sb(pm, "xacc", [128, 8, D], n=8)
            rowb = sb(pm, "rowb", [128, D])
            bl = sb(pm, "bl", [128, 128])
            lg = sb(pm, "lg", [128, 8, 36]); gatem = sb(pm, "gatem", [128, 8, 32])

            def bcast_rows(dst, colsrc, t, j0):
                for kg in range(4):
                    pb = PS[kg]
                    for kk in range(4):
                        k = kg * 4 + kk
                        OP("pool", "tensor_copy", bl[:], modT[:, j0 + k, t:t + 1].to_broadcast([128, 128]), r=[modT], w=[bl])
                        MM(pb[:, kk * 128:(kk + 1) * 128], bl[:], ident[:], r=[bl, ident], w=[pb])
                    OP("act", "copy", dst[:, kg * 512:(kg + 1) * 512], pb[:, 0:512], r=[pb], w=[dst])

            for i in range(8):
                DMA("sp", xacc[:, i, :], x_d[i * 128:(i + 1) * 128, :], w=[xacc.r[i]])
            with scope() as st:
                wo = [sb(st, "wo%d" % i, [128, 16, 512], BF16) for i in range(2)]
                tmpw = sb(st, "tmpw", [128, 512])
                bcast_rows(rowb, None, 0, 32)
                wov = wout_d.rearrange("(k p) n -> p k n", p=128)
                for dq in range(4):
                    wb = wo[dq % 2]
                    DMA("pool", wb[:], wov[:, :, dq * 512:(dq + 1) * 512], w=[wb])
                    for i in range(8):
                        pb = PS[4 + i % 2]
                        for k in range(16):
                            MM(pb[:, 0:512], catT[:, k, i * 128:(i + 1) * 128], wb[:, k, :], start=(k == 0), stop=(k == 15),
                               r=[catT.r[k], wb], w=[pb])
                        OP("dve", "tensor_tensor", tmpw[:], pb[:, 0:512], rowb[:, dq * 512:(dq + 1) * 512], op=ALU.mult, r=[pb, rowb], w=[tmpw])
                        OP("dve", "tensor_tensor", xacc[:, i, dq * 512:(dq + 1) * 512], xacc[:, i, dq * 512:(dq + 1) * 512], tmpw[:],
                           op=ALU.add, r=[tmpw, xacc.r[i]], w=[xacc.r[i]])
            DBG(xacc, xacc[:, 0, 0:512], 512)
            CHK(6)
            h2T = catT
            with scope() as st:
                a2b = sb(st, "a2b", [128, D]); sh2b = sb(st, "sh2b", [128, D]); h2f = sb(st, "h2f", [128, D])
                h2Tf = sb(st, "h2Tf", [128, 512]); wr = sb(st, "wr", [128, 16, 36]); brow = sb(st, "brow", [128, 36])
                junk = sb(st, "junk2", [128, D], BF16)
                DMA("sp", wr[:], wr_d.rearrange("(k p) n -> p k n", p=128), w=[wr]); DMA("sp", brow[:], brow_d, w=[brow])
                DMA("sp", h2f[:], g2_d, w=[h2f])
                bcast_rows(a2b, None, 0, 64)
                OP("dve", "scalar_tensor_tensor", a2b[:], a2b[:], 1.0, h2f[:], op0=ALU.add, op1=ALU.mult, r=[a2b, h2f], w=[a2b])
                bcast_rows(sh2b, None, 0, 48)
                for i in range(8):
                    c0 = small[:, 32:33]; c1 = small[:, 33:34]; c2 = small[:, 34:35]
                    OP("act", "activation", junk[:], xacc[:, i, :], AF.Square, accum_out=c0, r=[xacc.r[i]], w=[junk, small])
                    OP("act", "activation", c1, c0, AF.Sqrt, scale=1.0 / D, bias=EPS, r=[small], w=[small])
                    OP("dve", "reciprocal", c2, c1, r=[small], w=[small])
                    OP("dve", "scalar_tensor_tensor", h2f[:], xacc[:, i, :], c2, a2b[:], op0=ALU.mult, op1=ALU.mult,
                       r=[xacc.r[i], small, a2b], w=[h2f])
                    OP("dve", "tensor_tensor", h2f[:], h2f[:], sh2b[:], op=ALU.add, r=[h2f, sh2b], w=[h2f])
                    plg = PS[7]
                    for kg in range(4):
                        pb = PS[kg]
                        for kk in range(4):
                            k = kg * 4 + kk
                            TR(pb[:, kk * 128:(kk + 1) * 128], h2f[:, k * 128:(k + 1) * 128], ident[:], r=[h2f, ident], w=[pb])
                        OP("act", "copy", h2Tf[:], pb[:, 0:512], r=[pb], w=[h2Tf])
                        OP("dve", "tensor_copy", h2T[:, kg * 4:(kg + 1) * 4, i * 128:(i + 1) * 128],
                           pb[:, 0:512].rearrange("p (k n) -> p k n", n=128), r=[pb], w=h2T.r[kg * 4:(kg + 1) * 4])
                        for kk in range(4):
                            k = kg * 4 + kk
                            MM(plg[:, 0:36], h2Tf[:, kk * 128:(kk + 1) * 128], wr[:, k, :], start=(k == 0), stop=(k == 15), r=[h2Tf, wr], w=[plg])
                    OP("dve", "tensor_tensor", lg[:, i, :], plg[:, 0:36], brow[:], op=ALU.add, r=[plg, brow], w=[lg])
                elm = sb(st, "elm", [128, 32]); oh = sb(st, "oh", [128, 64]); top8 = sb(st, "top8", [128, 8]); scr = sb(st, "scr", [128, 16])
                for i in range(8):
                    gl = lg[:, i, 0:4]
                    S_ = lambda a, b=None: scr[:, a:(a + 1 if b is None else b)]
                    OP("dve", "tensor_reduce", S_(0), gl, axis=AX.X, op=ALU.max, r=[lg], w=[scr])
                    OP("dve", "tensor_scalar", S_(1), S_(0), -1.0, None, op0=ALU.mult, r=[scr], w=[scr])
                    OP("dve", "tensor_scalar", S_(4, 8), gl, S_(0), None, op0=ALU.is_equal, r=[lg, scr], w=[scr])
                    OP("act", "activation", S_(8, 12), gl, AF.Exp, bias=S_(1), accum_out=S_(2), r=[lg, scr], w=[scr])
                    OP("dve", "reciprocal", S_(3), S_(2), r=[scr], w=[scr])
                    OP("dve", "tensor_scalar", S_(4, 8), S_(4, 8), 1.0, 1e30, op0=ALU.subtract, op1=ALU.mult, r=[scr], w=[scr])
                    for g in range(4):
                        OP("dve", "tensor_scalar", elm[:, g * 8:(g + 1) * 8], lg[:, i, 4 + g * 8:12 + g * 8], S_(4 + g), None, op0=ALU.add,
                           r=[lg, scr], w=[elm])
                    OP("dve", "max", out=top8[:], in_=elm[:], r=[elm], w=[top8])
                    OP("dve", "tensor_scalar", oh[:, 0:32], elm[:], top8[:, 0:1], None, op0=ALU.is_equal, r=[elm, top8], w=[oh])
                    OP("dve", "tensor_scalar", oh[:, 32:64], elm[:], top8[:, 1:2], None, op0=ALU.is_equal, r=[elm, top8], w=[oh])
                    OP("dve", "tensor_scalar", S_(12), top8[:, 0:1], -1.0, None, op0=ALU.mult, r=[top8], w=[scr])
                    OP("act", "activation", S_(13), top8[:, 1:2], AF.Exp, bias=S_(12), r=[top8, scr], w=[scr])
                    OP("dve", "tensor_scalar", S_(13), S_(13), 1.0, None, op0=ALU.add, r=[scr], w=[scr])
                    OP("dve", "reciprocal", S_(14), S_(13), r=[scr], w=[scr])
                    OP("dve", "tensor_tensor", S_(14), S_(14), S_(3), op=ALU.mult, r=[scr], w=[scr])
                    OP("dve", "tensor_tensor", S_(15), S_(3), S_(14), op=ALU.subtract, r=[scr], w=[scr])
                    OP("dve", "tensor_scalar", gatem[:, i, :], oh[:, 0:32], S_(14), None, op0=ALU.mult, r=[oh, scr], w=[gatem])
                    OP("dve", "scalar_tensor_tensor", gatem[:, i, :], oh[:, 32:64], S_(15), gatem[:, i, :], op0=ALU.mult, op1=ALU.add,
                       r=[oh, scr, gatem], w=[gatem])
            DBG(lg, lg[:, 0, :], 36); DBG(gatem, gatem[:, 0, :], 32)
            CHK(7)

            with scope() as st:
                NW = 4
                wring = [sb(st, "wring%d" % i, [128, 16, 256], BF16) for i in range(NW)]
                w2b = sb(st, "w2b", [128, 8, D], BF16)
                actT = sb(st, "actT", [128, 8, OWN], BF16, n=8)
                sg = [sb(st, "sg%d" % i, [128, 512]) for i in range(2)]
                ytmp = [sb(st, "ytmp%d" % i, [128, 512]) for i in range(2)]
                bcast_rows(rowb, None, 0, 80)
                pieces = []
                for e_ in range(NE):
                    w1v_ = w1_d[e_].rearrange("(k p) n -> p k n", p=128)
                    w3v_ = w3_d[e_].rearrange("(k p) n -> p k n", p=128)
                    for q_ in range(4):
                        pieces.append(w1v_[:, :, q_ * 256:(q_ + 1) * 256])
                        pieces.append(w3v_[:, :, q_ * 256:(q_ + 1) * 256])
                issued = [0]
                used = [0]

                def wload():
                    idx = used[0]
                    used[0] += 1
                    while issued[0] < min(len(pieces), idx + NW - 1):
                        p_ = issued[0]
                        issued[0] += 1
                        DMA("pool", wring[p_ % NW][:], pieces[p_], w=[wring[p_ % NW]])
                    return wring[idx % NW]

                hcnt = 0
                ycnt = 0
                for e in range(NE):
                    DMA("pool", w2b[:], w2_d[e].rearrange("(k p) n -> p k n", p=128), w=[w2b])
                    for q in range(4):
                        w1p = wload()
                        w3p = wload()
                        for f2 in range(2):
                            fc = q * 2 + f2
                            for nt in range(2):
                                ph1 = PS[hcnt % 2]; ph3 = PS[2 + hcnt % 2]; sgb = sg[hcnt % 2]
                                hcnt += 1
                                for dk in range(16):
                                    MM(ph1[:, 0:512], w1p[:, dk, f2 * 128:(f2 + 1) * 128], h2T[:, dk, nt * 512:(nt + 1) * 512],
                                       start=(dk == 0), stop=(dk == 15), r=[w1p, h2T.r[dk]], w=[ph1])
                                for dk in range(16):
                                    MM(ph3[:, 0:512], w3p[:, dk, f2 * 128:(f2 + 1) * 128], h2T[:, dk, nt * 512:(nt + 1) * 512],
                                       start=(dk == 0), stop=(dk == 15), r=[w3p, h2T.r[dk]], w=[ph3])
                                OP("act", "activation", sgb[:], ph1[:, 0:512], AF.Silu, r=[ph1], w=[sgb])
                                OP("dve", "tensor_tensor", actT[:, fc, nt * 512:(nt + 1) * 512], sgb[:], ph3[:, 0:512], op=ALU.mult,
                                   r=[sgb, ph3], w=[actT.r[fc]])
                    for i in range(8):
                        for dq in range(4):
                            py = PS[4 + ycnt % 3]; yt = ytmp[ycnt % 2]
                            ycnt += 1
                            for fc in range(8):
                                MM(py[:, 0:512], actT[:, fc, i * 128:(i + 1) * 128], w2b[:, fc, dq * 512:(dq + 1) * 512],
                                   start=(fc == 0), stop=(fc == 7), r=[actT.r[fc], w2b], w=[py])
                            OP("act", "activation", yt[:], py[:, 0:512], AF.Identity, scale=gatem[:, i, e:e + 1], r=[py, gatem], w=[yt])
                            OP("dve", "tensor_tensor", yt[:], yt[:], rowb[:, dq * 512:(dq + 1) * 512], op=ALU.mult, r=[yt, rowb], w=[yt])
                            OP("pool", "tensor_tensor", xacc[:, i, dq * 512:(dq + 1) * 512], xacc[:, i, dq * 512:(dq + 1) * 512], yt[:],
                               op=ALU.add, r=[yt, xacc.r[i]], w=[xacc.r[i]])

            DBG(xacc, xacc[:, 0, 0:512], 512)
            with scope() as st:
                fgb = sb(st, "fgb", [128, D]); junk = sb(st, "junk3", [128, D], BF16)
                ot = [sb(st, "ot%d" % i, [128, D]) for i in range(2)]
                DMA("sp", fgb[:], fg_d, w=[fgb])
                for i in range(8):
                    c0 = small[:, 40:41]; c1 = small[:, 41:42]; c2 = small[:, 42:43]
                    OP("act", "activation", junk[:], xacc[:, i, :], AF.Square, accum_out=c0, r=[xacc.r[i]], w=[junk, small])
                    OP("act", "activation", c1, c0, AF.Sqrt, scale=1.0 / D, bias=EPS, r=[small], w=[small])
                    OP("dve", "reciprocal", c2, c1, r=[small], w=[small])
                    OP("dve", "scalar_tensor_tensor", ot[i % 2][:], xacc[:, i, :], c2, fgb[:], op0=ALU.mult, op1=ALU.mult,
                       r=[xacc.r[i], small, fgb], w=[ot[i % 2]])
                    final_ops.append(DMA("sp", out_d[i * 128:(i + 1) * 128, :], ot[i % 2][:], r=[ot[i % 2]], w=[out_dram]))

        pass
    except _Stop:
        pass
    P.emit(final_ops)
    return nc, P.stats


def _fm(v, n):
    return np.ascontiguousarray(np.asarray(v, np.float32).reshape(n, 128).T)


def _pinv(rev):
    out = np.zeros((4, 16, 64), np.float32)
    for g, w in enumerate((2, 4, 8, 16)):
        lo = w // 2
        hi = w - lo

        def cnt(n):
            i = np.arange(n)
            return np.clip(i + hi, 0, n) - np.clip(i - lo, 0, n)
        nr = cnt(32).astype(np.float32)
        ncn = cnt(64).astype(np.float32)
        if rev:
            nr = nr[::-1]
            ncn = ncn[::-1]
        out[g] = 1.0 / (nr[:16, None] * ncn[None, :])
    return np.ascontiguousarray(np.broadcast_to(out.reshape(1, 4, OWN), (128, 4, OWN)))


def prep_core(inp, b, th):
    rev = th == 1
    f32 = lambda a: np.ascontiguousarray(np.asarray(a, np.float32))
    x = inp["x"][b]
    ctx = inp["ctx"][b]
    if rev:
        x = x[::-1]
        ctx = ctx[::-1]
    m = {"x": f32(x), "ctx": f32(ctx)}
    cc = np.zeros((128, 16, 2), np.float32)
    cc[:, :, 0] = _fm(inp["c"][b], 16)
    cc[:, :, 1] = _fm(inp["c_ctx"], 16)
    m["cc"] = cc
    m["w_mod"] = f32(inp["w_mod"][0])
    m["bmodT"] = _fm(inp["b_mod"][0], 96)
    m["g1T"] = _fm(inp["norm1_g"][0], 16)
    cols = list(range(1024))
    for h in range(8):
        for base in (1024, 2048, 3072, 4096):
            cols += list(range(base + h * 128, base + (h + 1) * 128))
    AB0 = 5120
    bf, bb, af, ab_ = [list(range(AB0 + 8 * i, AB0 + 8 * i + 8)) for i in range(4)]
    cols += (bb + bf + ab_ + af) if rev else (bf + bb + af + ab_)
    m["w_in"] = f32(inp["w_in"][0][:, cols])
    cwa = np.asarray(inp["conv_w"][0], np.float32)
    if rev:
        cwa = cwa[::-1]
    cw = cwa.reshape(5, 3, 8, 128).transpose(3, 2, 1, 0)
    m["cw"] = f32(cw)
    s0a, s1a = (inp["a_log_b"][0], inp["a_log_f"][0]) if rev else (inp["a_log_f"][0], inp["a_log_b"][0])
    s0d, s1d = (inp["dt_bias_b"][0], inp["dt_bias_f"][0]) if rev else (inp["dt_bias_f"][0], inp["dt_bias_b"][0])
    m["alog_rep"] = f32(np.broadcast_to(np.concatenate([s0a, s1a])[None, None, :], (128, 18, 16)))
    m["dtb_rep"] = f32(np.broadcast_to(np.concatenate([s0d, s1d])[None, None, :], (128, 18, 16)))
    m["ong"] = f32(np.asarray(inp["out_norm_g"][0]).reshape(128, 1))
    m["pool_w"] = f32(inp["pool_w"][0])
    m["psT"] = _fm(inp["pool_scale"][0], 8)
    m["pinv"] = _pinv(rev)
    m["flipc"] = f32(np.broadcast_to(np.array([[0.0, 1.0]] if rev else [[1.0, 0.0]], np.float32), (128, 2)))
    m["w_out"] = f32(inp["w_out"][0])
    m["g2_bc"] = f32(np.broadcast_to(np.asarray(inp["norm2_g"][0])[None, :], (128, D)))
    m["fg_bc"] = f32(np.broadcast_to(np.asarray(inp["final_g"])[None, :], (128, D)))
    m["wr"] = f32(np.concatenate([inp["w_grp"][0], inp["w_rt"][0]], axis=1))
    m["brow"] = f32(np.broadcast_to(np.concatenate([inp["b_grp"][0], inp["b_rt"][0]])[None, :], (128, 36)))
    m["w1"] = f32(inp["w1"][0])
    m["w3"] = f32(inp["w3"][0])
    m["w2"] = f32(inp["w2"][0])
    return m


_CACHE = {}


def kernel(**inputs):
    inp = {k: np.asarray(v) for k, v in inputs.items()}
    if "nc" not in _CACHE:
        _CACHE["nc"] = build()[0]
    nc = _CACHE["nc"]
    shared = {}
    in_maps = []
    for core in range(8):
        b, th = core // 2, core % 2
        m = prep_core(inp, b, th)
        for k in ("w_mod", "w_out", "w1", "w3", "w2", "pool_w", "wr"):
            if k in shared:
                m[k] = shared[k]
            else:
                shared[k] = m[k]
        in_maps.append(m)
    res = run_bass_kernel_spmd(nc, in_maps, core_ids=list(range(8)))
    out = np.zeros((4, T, D), np.float32)
    for core in range(8):
        b, th = core // 2, core % 2
        o = np.asarray(res.results[core]["out"], np.float32)
        if th == 0:
            out[b, 0:OWN] = o
        else:
            out[b, T - OWN:T] = o[::-1]
    return out
```

```python
import contextlib
import numpy as np
import concourse.bass as bass
import concourse.mybir as mybir
from concourse.bass_utils import run_bass_kernel_spmd

F32 = mybir.dt.float32
BF16 = mybir.dt.bfloat16
ALU = mybir.AluOpType
AF = mybir.ActivationFunctionType
AX = mybir.AxisListType

D = 2048
T = 2048
TC = 256
OWN = 1024
NE = 32
EPS = 1e-6
CAP = 128


class Res:
    __slots__ = ("name", "lw", "rd", "psum")

    def __init__(self, name="", psum=False):
        self.name = name
        self.lw = None
        self.rd = []
        self.psum = psum


class Op:
    __slots__ = ("eng", "fn", "waits", "signal", "seq", "dma", "dsem", "dval")

    def __init__(self, eng, fn, dma):
        self.eng = eng
        self.fn = fn
        self.waits = []
        self.signal = False
        self.seq = 0
        self.dma = dma
        self.dsem = None
        self.dval = 0


class Prog:
    def __init__(self, nc, n_dma_sems=16):
        self.nc = nc
        self.ops = []
        self.n_dma_sems = n_dma_sems
        self.dma_rr = 0
        self.dma_cnt = [0] * n_dma_sems
        self.bar = None
        self.last = {}
        self.last_dma = {}

    def barrier(self, fn):
        o = Op("pool", fn, False)
        for e, d in self.last.items():
            if e != "pool":
                o.waits.append(d)
                d.signal = True
        for sidx, d in self.last_dma.items():
            o.waits.append(d)
        if self.bar is not None:
            o.waits.append(self.bar)
        o.signal = True
        self.ops.append(o)
        self.last["pool"] = o
        self.bar = o
        return o

    def _add(self, op, reads, writes):
        for r in list(reads):
            if r.psum:
                writes.append(r)
        if op.dma:
            self.last_dma[op.dsem] = op
        else:
            self.last[op.eng] = op
        if self.bar is not None and (op.dma or op.eng != "pool"):
            op.waits.append(self.bar)
        deps = []
        for r in reads:
            if r.lw is not None:
                deps.append((r.lw, True))
        for w in writes:
            if w.lw is not None:
                deps.append((w.lw, False))
            for o in w.rd:
                deps.append((o, False))
        for (d, raw) in deps:
            if d is op:
                continue
            if d.dma:
                op.waits.append(d)
                d.signal = True
            elif d.eng == op.eng and not op.dma:
                if (raw and op.eng != "pe") or op.eng == "pool":
                    op.waits.append(d)
                    d.signal = True
            else:
                op.waits.append(d)
                d.signal = True
        for r in reads:
            r.rd.append(op)
        for w in writes:
            w.lw = op
            w.rd = []
        self.ops.append(op)
        return op

    def op(self, eng, fn, reads=(), writes=()):
        return self._add(Op(eng, fn, False), list(reads), list(writes))

    def dma(self, eng, fn, reads=(), writes=()):
        o = Op(eng, fn, True)
        s = self.dma_rr
        self.dma_rr = (self.dma_rr + 1) % self.n_dma_sems
        self.dma_cnt[s] += 16
        o.dsem = s
        o.dval = self.dma_cnt[s]
        prev = self.last_dma.get(s)
        self._add(o, list(reads), list(writes))
        if prev is not None:
            o.waits.append(prev)
        return o

    def emit(self, final_wait_ops=()):
        nc = self.nc
        engs = {"pe": nc.tensor, "act": nc.scalar, "dve": nc.vector, "pool": nc.gpsimd, "sp": nc.sync}
        with contextlib.ExitStack() as st:
            csem = {e: st.enter_context(nc.semaphore("c_" + e)) for e in engs}
            dsem = [st.enter_context(nc.semaphore("d_%d" % i)) for i in range(self.n_dma_sems)]
            cnt = {e: 0 for e in csem}
            for o in self.ops:
                if o.signal and not o.dma:
                    cnt[o.eng] += 1
                    o.seq = cnt[o.eng]
            seen = {e: {} for e in engs}
            nwait = 0
            for o in self.ops:
                E = engs[o.eng]
                need = {}
                for d in o.waits:
                    key = ("d", d.dsem) if d.dma else ("c", d.eng)
                    val = d.dval if d.dma else d.seq
                    if need.get(key, 0) < val:
                        need[key] = val
                for key, val in need.items():
                    if seen[o.eng].get(key, 0) >= val:
                        continue
                    seen[o.eng][key] = val
                    sem = dsem[key[1]] if key[0] == "d" else csem[key[1]]
                    E.wait_ge(sem, val)
                    nwait += 1
                ins = o.fn()
                if o.dma:
                    ins.then_inc(dsem[o.dsem], 16)
                elif o.signal:
                    ins.then_inc(csem[o.eng], 1)
            need = {}
            for d in final_wait_ops:
                key = ("d", d.dsem) if d.dma else ("c", d.eng)
                val = d.dval if d.dma else d.seq
                need[key] = max(need.get(key, 0), val)
            for key, val in need.items():
                sem = dsem[key[1]] if key[0] == "d" else csem[key[1]]
                nc.sync.wait_ge(sem, val)
            self.stats = dict(n_ops=len(self.ops), n_wait=nwait, cnt=cnt, dma=list(self.dma_cnt))


class _Stop(Exception):
    pass


class Tl:
    def __init__(self, t, n=1, psum=False):
        self.t = t
        self.r = [Res(psum=psum) for _ in range(n)]

    def __getitem__(self, idx):
        return self.t[idx]


def build(dbg=None, stop=None):
    nc = bass.Bass("TRN2", target_bir_lowering=False)
    P = Prog(nc)

    def din(name, shape):
        return nc.dram_tensor(name, list(shape), F32, kind="ExternalInput").ap()

    x_d = din("x", [T, D]); ctx_d = din("ctx", [TC, D]); cc_d = din("cc", [128, 16, 2])
    wmod_d = din("w_mod", [D, 6 * D]); bmod_d = din("bmodT", [128, 96]); g1_d = din("g1T", [128, 16])
    win_d = din("w_in", [D, 5152]); cw_d = din("cw", [128, 8, 3, 5])
    alog_d = din("alog_rep", [128, 18, 16]); dtb_d = din("dtb_rep", [128, 18, 16])
    ong_d = din("ong", [128, 1]); poolw_d = din("pool_w", [4, 256, 256]); ps_d = din("psT", [128, 8])
    pinv_d = din("pinv", [128, 4, OWN]); flip_d = din("flipc", [128, 2])
    wout_d = din("w_out", [D, D]); g2_d = din("g2_bc", [128, D]); fg_d = din("fg_bc", [128, D])
    wr_d = din("wr", [D, 36]); brow_d = din("brow", [128, 36])
    w1_d = din("w1", [NE, D, 1024]); w3_d = din("w3", [NE, D, 1024]); w2_d = din("w2", [NE, 1024, D])
    out_d = nc.dram_tensor("out", [OWN, D], F32, kind="ExternalOutput").ap()
    dbg_d = None
    if dbg:
        dbg_d = nc.dram_tensor("dbg", [128, dbg], F32, kind="ExternalOutput").ap()
    dbg_off = [0]
    out_dram = Res("out")
    dbg_res = Res("dbg")

    E = {"pe": nc.tensor, "act": nc.scalar, "dve": nc.vector, "pool": nc.gpsimd}

    def rs(tls):
        out = []
        for t in tls:
            if isinstance(t, Tl):
                out.extend(t.r)
            elif isinstance(t, Res):
                out.append(t)
            else:
                out.extend(t)
        return out

    def OP(eng, name, *args, r=(), w=(), **kw):
        return P.op(eng, lambda: getattr(E[eng], name)(*args, **kw), rs(r), rs(w))

    def MM(out, lhsT, rhs, start=True, stop=True, r=(), w=()):
        return P.op("pe", lambda: nc.tensor.matmul(out, lhsT, rhs, start=start, stop=stop), rs(r), rs(w))

    def TR(out, in_, ident_ap, r=(), w=()):
        return P.op("pe", lambda: nc.tensor.transpose(out, in_, ident_ap), rs(r), rs(w))

    def DMA(q, out, in_, r=(), w=()):
        eng = {"sp": nc.sync, "pool": nc.gpsimd, "act": nc.scalar}[q]
        return P.dma(q, lambda: eng.dma_start(out=out, in_=in_), rs(r), rs(w))

    final_ops = []

    def DBG(tl, ap, ncols):
        if not dbg:
            return
        o = dbg_off[0]
        dbg_off[0] += ncols
        assert dbg_off[0] <= dbg
        final_ops.append(DMA("sp", dbg_d[:, o:o + ncols], ap, r=[tl], w=[dbg_res]))

    try:
     with contextlib.ExitStack() as top:
        def sb(st, name, shape, dt=F32, n=1):
            return Tl(st.enter_context(nc.sbuf_tensor("s_" + name, list(shape), dt)), n)

        PS = [Tl(top.enter_context(nc.psum_tensor("ps%d" % i, [128, 512], F32)), psum=True) for i in range(8)]
        bscr = sb(top, "bscr", [128, 1])

        def BAR():
            P.barrier(lambda: nc.gpsimd.memset(bscr[:], 0.0))

        def CHK(k):
            if stop == k:
                raise _Stop()

        @contextlib.contextmanager
        def scope():
            BAR()
            with contextlib.ExitStack() as st_:
                yield st_
            BAR()

        ident = sb(top, "ident", [128, 128]); ones = sb(top, "ones", [128, 128])
        mL = sb(top, "mL", [128, 128]); mU = sb(top, "mU", [128, 128])
        mLs = sb(top, "mLs", [128, 128]); mUs = sb(top, "mUs", [128, 128])
        iota = sb(top, "iota", [128, 128])
        OP("pool", "memset", ones[:], 1.0, w=[ones])
        OP("pool", "memset", ident[:], 0.0, w=[ident])
        OP("pool", "affine_select", ident[:], ident[:], pattern=[[-1, 128]], compare_op=ALU.not_equal, fill=1.0,
           base=0, channel_multiplier=1, r=[ident], w=[ident])
        OP("pool", "affine_select", mL[:], ones[:], pattern=[[-1, 128]], compare_op=ALU.is_ge, fill=0.0,
           base=0, channel_multiplier=1, r=[ones], w=[mL])
        OP("pool", "affine_select", mLs[:], ones[:], pattern=[[-1, 128]], compare_op=ALU.is_gt, fill=0.0,
           base=0, channel_multiplier=1, r=[ones], w=[mLs])
        OP("pool", "affine_select", mU[:], ones[:], pattern=[[1, 128]], compare_op=ALU.is_ge, fill=0.0,
           base=0, channel_multiplier=-1, r=[ones], w=[mU])
        OP("pool", "affine_select", mUs[:], ones[:], pattern=[[1, 128]], compare_op=ALU.is_gt, fill=0.0,
           base=0, channel_multiplier=-1, r=[ones], w=[mUs])
        OP("pool", "tensor_scalar", mLs[:], mLs[:], -1.0, None, op0=ALU.mult, r=[mLs], w=[mLs])
        OP("pool", "tensor_scalar", mUs[:], mUs[:], -1.0, None, op0=ALU.mult, r=[mUs], w=[mUs])
        OP("pool", "iota", iota[:], pattern=[[1, 128]], base=0, channel_multiplier=0,
           allow_small_or_imprecise_dtypes=True, w=[iota])

        modT = sb(top, "modT", [128, 96, 2])
        catT = sb(top, "catT", [128, 16, OWN], BF16, n=16)
        small = sb(top, "small", [128, 64])
        ong = sb(top, "ong", [128, 1]); flipc = sb(top, "flipc", [128, 2]); psT = sb(top, "psT", [128, 8])
        DMA("sp", ong[:], ong_d, w=[ong]); DMA("sp", flipc[:], flip_d, w=[flipc]); DMA("sp", psT[:], ps_d, w=[psT])

        DBG(modT, modT[:].rearrange("p j t -> p (j t)"), 192)
        CHK(1)

        def modc(j, t):
            return modT[:, j, t:t + 1]

        with scope() as mx:
            h1T = sb(mx, "h1T", [128, 16, T + TC], BF16, n=18)
            a1 = sb(mx, "a1", [128, 16, 2]); g1T = sb(mx, "g1T", [128, 16]); sh1c = sb(mx, "sh1c", [128, 16, 2])
            DMA("sp", g1T[:], g1_d, w=[g1T])
            with scope() as st:
                cc = sb(st, "cc", [128, 16, 2]); scc = sb(st, "scc", [128, 16, 2]); bmod = sb(st, "bmod", [128, 96])
                wm = [sb(st, "wm%d" % i, [128, 16, 512]) for i in range(2)]
                xt = [sb(st, "xt%d" % i, [128, D]) for i in range(2)]
                junk = sb(st, "junk", [128, D], BF16)
                DMA("sp", cc[:], cc_d, w=[cc]); DMA("sp", bmod[:], bmod_d, w=[bmod])
                OP("act", "activation", scc[:], cc[:], AF.Silu, r=[cc], w=[scc])
                wmv = wmod_d.rearrange("(k p) n -> p k n", p=128)

                def mod_group(jg, psm):
                    wb = wm[jg % 2]
                    DMA("sp" if jg % 2 == 0 else "act", wb[:], wmv[:, :, jg * 512:(jg + 1) * 512], w=[wb])
                    for jc in range(4):
                        j = jg * 4 + jc
                        for k in range(16):
                            MM(psm[:, 2 * j:2 * j + 2], wb[:, k, jc * 128:(jc + 1) * 128], scc[:, k, :],
                               start=(k == 0), stop=(k == 15), r=[wb, scc], w=[psm])

                def mod_evac(psm, j0, j1):
                    pv = psm[:, 2 * j0:2 * j1].rearrange("p (j t) -> p j t", t=2)
                    for t in range(2):
                        OP("dve", "tensor_tensor", modT[:, j0:j1, t], pv[:, :, t], bmod[:, j0:j1], op=ALU.add, r=[psm, bmod], w=[modT])

                for jg in range(8):
                    mod_group(jg, PS[4])
                mod_evac(PS[4], 0, 32)
                for t in range(2):
                    OP("dve", "scalar_tensor_tensor", a1[:, :, t], modT[:, 16:32, t], 1.0, g1T[:], op0=ALU.add, op1=ALU.mult,
                       r=[modT, g1T], w=[a1])
                OP("dve", "tensor_copy", sh1c[:], modT[:, 0:16, :], r=[modT], w=[sh1c])
                for i in range(18):
                    xb = xt[i % 2]
                    src = x_d[i * 128:(i + 1) * 128, :] if i < 16 else ctx_d[(i - 16) * 128:(i - 15) * 128, :]
                    t = 0 if i < 16 else 1
                    DMA("sp", xb[:], src, w=[xb])
                    c0 = small[:, 0:1]; c1 = small[:, 1:2]; c2 = small[:, 2:3]
                    OP("act", "activation", junk[:], xb[:], AF.Square, accum_out=c0, r=[xb], w=[junk, small])
                    OP("act", "activation", c1, c0, AF.Sqrt, scale=1.0 / D, bias=EPS, r=[small], w=[small])
                    OP("dve", "reciprocal", c2, c1, r=[small], w=[small])
                    OP("dve", "tensor_scalar", xb[:], xb[:], c2, None, op0=ALU.mult, r=[xb, small], w=[xb])
                    for kg in range(4):
                        pb = PS[kg]
                        for kk in range(4):
                            k = kg * 4 + kk
                            TR(pb[:, kk * 128:(kk + 1) * 128], xb[:, k * 128:(k + 1) * 128], ident[:], r=[xb, ident], w=[pb])
                        for kk in range(4):
                            k = kg * 4 + kk
                            dst = h1T[:, k, i * 128:(i + 1) * 128]
                            if kk % 2 == 0:
                                OP("act", "activation", dst, pb[:, kk * 128:(kk + 1) * 128], AF.Identity,
                                   scale=a1[:, k, t:t + 1], bias=sh1c[:, k, t:t + 1], r=[pb, a1, sh1c], w=[h1T.r[i]])
                            else:
                                OP("dve", "tensor_scalar", dst, pb[:, kk * 128:(kk + 1) * 128], a1[:, k, t:t + 1], sh1c[:, k, t:t + 1],
                                   op0=ALU.mult, op1=ALU.add, r=[pb, a1, sh1c], w=[h1T.r[i]])
                    if i < 16:
                        mod_group(8 + i, PS[5])
                mod_evac(PS[5], 32, 96)
            CHK(2)
            winv = win_d.rearrange("(k p) n -> p k n", p=128)
            wpc = [sb(mx, "wpc%d" % i, [128, 16, 128], BF16) for i in range(1)]
            wp_i = [0]

            def load_w(col0, ncol=128):
                wb = wpc[0]
                wp_i[0] += 1
                DMA("pool", wb[:, :, 0:ncol], winv[:, :, col0:col0 + ncol], w=[wb])
                return wb

            def proj(wb, M, tok0, n, pbank):
                tiles = h1T.r[tok0 // 128:(tok0 + n + 127) // 128]
                for k in range(16):
                    MM(pbank[0:M, 0:n], wb[:, k, 0:M], h1T[:, k, tok0:tok0 + n], start=(k == 0), stop=(k == 15),
                       r=[wb] + tiles, w=[pbank])

            NTL = 18
            gates = {}
            for nm in ("beta", "gc", "ekd", "bg", "gtot"):
                gates[nm] = sb(mx, "g_" + nm, [128, NTL, 16])
            with scope() as st:
                abT = sb(st, "abT", [32, T + TC]); ab_tok = sb(st, "ab_tok", [128, NTL, 32])
                alog = sb(st, "alog", [128, NTL, 16]); dtb = sb(st, "dtb", [128, NTL, 16])
                gtk = sb(st, "gtk", [128, NTL, 16]); tmpg = sb(st, "tmpg", [128, NTL, 16]); tot = sb(st, "tot", [128, NTL, 16])
                DMA("sp", alog[:], alog_d, w=[alog]); DMA("sp", dtb[:], dtb_d, w=[dtb])
                wb = load_w(5120, 32)
                for nt in range(5):
                    n = 512 if nt < 4 else 256
                    pb = PS[nt % 4]
                    proj(wb, 32, nt * 512, n, pb)
                    OP("act", "copy", abT[0:32, nt * 512:nt * 512 + n], pb[0:32, 0:n], r=[pb], w=[abT])
                pb = PS[4]
                for i in range(NTL):
                    TR(pb[:, (i % 16) * 32:(i % 16) * 32 + 32], abT[0:32, i * 128:(i + 1) * 128], ident[0:32, 0:32], r=[abT, ident], w=[pb])
                    if i == 15:
                        OP("act", "copy", ab_tok[:, 0:16, :], pb[:, 0:512].rearrange("p (i c) -> p i c", c=32), r=[pb], w=[ab_tok])
                OP("act", "copy", ab_tok[:, 16:18, :], pb[:, 0:64].rearrange("p (i c) -> p i c", c=32), r=[pb], w=[ab_tok])
                OP("act", "activation", gates["beta"][:], ab_tok[:, :, 0:16], AF.Sigmoid, r=[ab_tok], w=[gates["beta"]])
                OP("dve", "tensor_tensor", tmpg[:], ab_tok[:, :, 16:32], dtb[:], op=ALU.add, r=[ab_tok, dtb], w=[tmpg])
                OP("act", "activation", tmpg[:], tmpg[:], AF.Exp, r=[tmpg], w=[tmpg])
                OP("act", "activation", tmpg[:], tmpg[:], AF.Ln, bias=1.0, r=[tmpg], w=[tmpg])
                OP("act", "activation", alog[:], alog[:], AF.Exp, r=[alog], w=[alog])
                OP("dve", "scalar_tensor_tensor", gtk[:], tmpg[:], -1.0, alog[:], op0=ALU.mult, op1=ALU.mult, r=[tmpg, alog], w=[gtk])
                pg = PS[5]; pt = PS[6]
                for i in range(NTL):
                    MM(pg[:, i * 16:i * 16 + 8], mU[:], gtk[:, i, 0:8], r=[mU, gtk], w=[pg])
                    MM(pg[:, i * 16 + 8:i * 16 + 16], mL[:], gtk[:, i, 8:16], r=[mL, gtk], w=[pg])
                    MM(pt[:, i * 16:i * 16 + 16], ones[:], gtk[:, i, :], r=[ones, gtk], w=[pt])
                gc = gates["gc"]
                OP("act", "copy", gc[:], pg[:, 0:NTL * 16].rearrange("p (i c) -> p i c", c=16), r=[pg], w=[gc])
                OP("act", "copy", tot[:], pt[:, 0:NTL * 16].rearrange("p (i c) -> p i c", c=16), r=[pt], w=[tot])
                egc_t = sb(st, "egc_t", [128, NTL, 16])
                OP("act", "activation", egc_t[:], gc[:], AF.Exp, r=[gc], w=[egc_t])
                OP("act", "activation", gates["gtot"][:], tot[:], AF.Exp, r=[tot], w=[gates["gtot"]])
                OP("dve", "tensor_tensor", tmpg[:], tot[:], gc[:], op=ALU.subtract, r=[tot, gc, tmpg], w=[tmpg])
                OP("act", "activation", gates["ekd"][:], tmpg[:], AF.Exp, r=[tmpg], w=[gates["ekd"]])
                OP("dve", "tensor_tensor", gates["bg"][:], gates["beta"][:], egc_t[:], op=ALU.mult,
                   r=[gates["beta"], egc_t], w=[gates["bg"]])

            CHK(3)
            with scope() as hs:
                A = sb(hs, "A", [128, 2312]); qT = sb(hs, "qT", [128, OWN + TC]); kT = sb(hs, "kT", [128, T + TC])
                vT = sb(hs, "vT", [128, T + TC]); k_tok = sb(hs, "k_tok", [128, NTL, 128], n=NTL); v_tok = sb(hs, "v_tok", [128, NTL, 128], n=NTL)
                cw = sb(hs, "cw", [128, 8, 3, 5]); o_acc = sb(hs, "o_acc", [128, 8, 128], n=8)
                sq = sb(hs, "sq", [128, 256]); rt = sb(hs, "rt", [128, 256])
                NS = 2
                uw = [[sb(hs, "uw%d_%d" % (s, i), [128, 256]) for i in range(NS)] for s in range(2)]
                kdec = [[sb(hs, "kd%d_%d" % (s, i), [128, 128]) for i in range(NS)] for s in range(2)]
                qkT = [[sb(hs, "qk%d_%d" % (s, i), [128, 128]) for i in range(NS)] for s in range(2)]
                qdT = [[sb(hs, "qd%d_%d" % (s, i), [128, 128]) for i in range(NS)] for s in range(2)]
                Pb = [[sb(hs, "P%d_%d" % (s, i), [128, 128]) for i in range(2)] for s in range(2)]
                QR = [[sb(hs, "QR%d_%d" % (s, i), [128, 256]) for i in range(2)] for s in range(2)]
                gcb = [sb(hs, "gcb%d" % s, [128, 128]) for s in range(2)]
                tA = [sb(hs, "tA%d" % s, [128, 128]) for s in range(2)]; tB = [sb(hs, "tB%d" % s, [128, 128]) for s in range(2)]
                E1 = [sb(hs, "E1%d" % s, [128, 128]) for s in range(2)]; E2 = [sb(hs, "E2%d" % s, [128, 128]) for s in range(2)]
                Eg = tA
                GE = [sb(hs, "GE%d" % s, [128, 128]) for s in range(2)]
                TB = [sb(hs, "TB%d" % s, [128, 128]) for s in range(2)]; TBG = [sb(hs, "TBG%d" % s, [128, 128]) for s in range(2)]
                Sst = [[sb(hs, "S%d_%d" % (s, i), [128, 128]) for i in range(2)] for s in range(2)]
                vnew = [sb(hs, "vn%d" % s, [128, 128]) for s in range(2)]
                DMA("sp", cw[:], cw_d, w=[cw])
                OP("pool", "memset", A[:], 0.0, w=[A])

                def conv(dst, dcol, src0, n, h, j):
                    o = dst[:, dcol:dcol + n]
                    OP("dve", "tensor_scalar", o, A[:, src0:src0 + n], cw[:, h, j, 0:1], None, op0=ALU.mult, r=[A, cw], w=[dst])
                    for tap in range(1, 5):
                        OP("dve", "scalar_tensor_tensor", o, A[:, src0 + tap:src0 + tap + n], cw[:, h, j, tap:tap + 1], o,
                           op0=ALU.mult, op1=ALU.add, r=[A, cw, dst], w=[dst])
                    OP("act", "activation", o, o, AF.Silu, r=[dst], w=[dst])

                def l2norm(dst, col0, n, scale, bias):
                    if n > 256:
                        for o_ in range(0, n, 256):
                            l2norm(dst, col0 + o_, 256, scale, bias)
                        return
                    o = dst[:, col0:col0 + n]
                    pb = PS[7]
                    OP("act", "activation", sq[:, 0:n], o, AF.Square, r=[dst], w=[sq])
                    MM(pb[:, 0:n], ones[:], sq[:, 0:n], r=[ones, sq], w=[pb])
                    OP("act", "activation", rt[:, 0:n], pb[:, 0:n], AF.Sqrt, scale=scale, bias=bias, r=[pb], w=[rt])
                    OP("dve", "reciprocal", rt[:, 0:n], rt[:, 0:n], r=[rt], w=[rt])
                    OP("dve", "tensor_tensor", o, o, rt[:, 0:n], op=ALU.mult, r=[dst, rt], w=[dst])

                for h in range(8):
                    cbase = 1024 + h * 512
                    wb = load_w(cbase)
                    for (tok0, n, a0) in ((0, 512, 2), (512, 512, 514), (1024, 128, 1026), (T, 256, 2054)):
                        pb = PS[(tok0 // 512) % 4]
                        proj(wb, 128, tok0, n, pb)
                        OP("act", "copy", A[:, a0:a0 + n], pb[:, 0:n], r=[pb], w=[A])
                    conv(qT, 0, 0, OWN, h, 0)
                    conv(qT, OWN, 2052, TC, h, 0)
                    for c0 in (0, 512, 1024):
                        l2norm(qT, c0, 512 if c0 < 1024 else 256, 128.0, 128.0 * EPS)
                    wb = load_w(cbase + 128)
                    for nt in range(5):
                        n = 512 if nt < 4 else 256
                        a0 = 2 + nt * 512 if nt < 4 else 2054
                        pb = PS[nt % 4]
                        proj(wb, 128, nt * 512, n, pb)
                        OP("act", "copy", A[:, a0:a0 + n], pb[:, 0:n], r=[pb], w=[A])
                    conv(kT, 0, 0, T, h, 1)
                    conv(kT, T, 2052, TC, h, 1)
                    for nt in range(5):
                        l2norm(kT, nt * 512, 512 if nt < 4 else 256, 1.0, EPS)
                    for g in range(5):
                        pb = PS[g % 4]
                        ng = 4 if g < 4 else 2
                        for ii in range(ng):
                            i = g * 4 + ii
                            TR(pb[:, ii * 128:(ii + 1) * 128], kT[:, i * 128:(i + 1) * 128], ident[:], r=[kT, ident], w=[pb])
                        OP("act", "copy", k_tok[:, g * 4:g * 4 + ng, :], pb[:, 0:ng * 128].rearrange("p (i c) -> p i c", c=128),
                           r=[pb], w=k_tok.r[g * 4:g * 4 + ng])
                    wb = load_w(cbase + 256)
                    for nt in range(5):
                        n = 512 if nt < 4 else 256
                        a0 = 2 + nt * 512 if nt < 4 else 2054
                        pb = PS[nt % 4]
                        proj(wb, 128, nt * 512, n, pb)
                        OP("act", "copy", A[:, a0:a0 + n], pb[:, 0:n], r=[pb], w=[A])
                    conv(vT, 0, 0, T, h, 2)
                    conv(vT, T, 2052, TC, h, 2)
                    for g in range(5):
                        pb = PS[g % 4]
                        ng = 4 if g < 4 else 2
                        for ii in range(ng):
                            i = g * 4 + ii
                            TR(pb[:, ii * 128:(ii + 1) * 128], vT[:, i * 128:(i + 1) * 128], ident[:], r=[vT, ident], w=[pb])
                        OP("act", "copy", v_tok[:, g * 4:g * 4 + ng, :], pb[:, 0:ng * 128].rearrange("p (i c) -> p i c", c=128),
                           r=[pb], w=v_tok.r[g * 4:g * 4 + ng])
                    wb = load_w(cbase + 384)
                    for nt in range(2):
                        pb = PS[nt]
                        proj(wb, 128, nt * 512, 512, pb)
                        OP("act", "activation", catT[:, 8 + h, nt * 512:(nt + 1) * 512], pb[:, 0:512], AF.Silu, r=[pb], w=[catT.r[8 + h]])
                    if h == 0:
                        DBG(qT, qT[:, 0:256], 256); DBG(kT, kT[:, 0:256], 256); DBG(vT, vT[:, 0:256], 256)
                        DBG(k_tok, k_tok[:, 16, :], 128)

                    def prep(s, i, slot, want_out):
                        j = s * 8 + h
                        col = lambda nm: gates[nm][:, i, j:j + 1]
                        M1s = mLs if s == 0 else mUs
                        M2 = mU if s == 0 else mL
                        kc = kT[:, i * 128:(i + 1) * 128]
                        pA = PS[s]; pD = PS[2 + s]
                        OP("pool", "tensor_copy", gcb[s][:], col("gc").to_broadcast([128, 128]), r=[gates["gc"]], w=[gcb[s]])
                        MM(pA[:, 0:128], gcb[s][:], ident[:], r=[gcb[s], ident], w=[pA])
                        MM(pA[:, 128:256], kc, kc, r=[kT], w=[pA])
                        if want_out:
                            qc = qT[:, i * 128:(i + 1) * 128]
                            MM(pA[:, 256:384], kc, qc, r=[kT, qT], w=[pA])
                        yield
                        OP("dve", "tensor_scalar", tA[s][:], pA[:, 0:128], col("gc"), 0.0, op0=ALU.subtract, op1=ALU.max,
                           r=[pA, gates["gc"]], w=[tA[s]])
                        if want_out:
                            OP("dve", "tensor_scalar", tB[s][:], pA[:, 0:128], col("gc"), 0.0, op0=ALU.subtract, op1=ALU.min,
                               r=[pA, gates["gc"]], w=[tB[s]])
                        OP("act", "activation", E1[s][:], tA[s][:], AF.Exp, scale=-1.0, r=[tA[s]], w=[E1[s]])
                        if want_out:
                            OP("act", "activation", E2[s][:], tB[s][:], AF.Exp, r=[tB[s]], w=[E2[s]])
                            OP("act", "activation", Eg[s][:], pA[:, 0:128], AF.Exp, r=[pA], w=[Eg[s]])
                        yield
                        OP("dve", "tensor_tensor", GE[s][:], pA[:, 128:256], E1[s][:], op=ALU.mult, r=[pA, E1[s]], w=[GE[s]])
                        P0 = Pb[s][0]; QR0 = QR[s][0]
                        OP("dve", "scalar_tensor_tensor", P0[:], GE[s][:], col("beta"), M1s[:], op0=ALU.mult, op1=ALU.mult,
                           r=[GE[s], gates["beta"], M1s], w=[P0])
                        if want_out:
                            OP("pool", "tensor_tensor", E2[s][:], E2[s][:], M2[:], op=ALU.mult, r=[E2[s], M2], w=[E2[s]])
                            OP("dve", "tensor_tensor", qkT[s][slot][:], pA[:, 256:384], E2[s][:], op=ALU.mult, r=[pA, E2[s]], w=[qkT[s][slot]])
                            OP("pool", "tensor_tensor", qdT[s][slot][:], qT[:, i * 128:(i + 1) * 128], Eg[s][:], op=ALU.mult,
                               r=[qT, Eg[s]], w=[qdT[s][slot]])
                        yield
                        TR(pA[:, 384:512], P0[:], ident[:], r=[P0, ident], w=[pA])
                        yield
                        OP("act", "copy", QR0[:, 0:128], pA[:, 384:512], r=[pA], w=[QR0])
                        yield
                        pd = pD
                        MM(pd[:, 0:128], P0[:], QR0[:, 0:128], r=[P0, QR0], w=[pd])
                        MM(pd[:, 256:384], QR0[:, 0:128], P0[:], r=[P0, QR0], w=[pd])
                        yield
                        P1 = Pb[s][1]; QR1 = QR[s][1]
                        OP("act", "copy", QR1[:, 0:128], pd[:, 0:128], r=[pd], w=[QR1])
                        OP("dve", "tensor_copy", P1[:], pd[:, 256:384], r=[pd], w=[P1])
                        OP("pool", "tensor_tensor", QR1[:, 128:256], QR0[:, 0:128], ident[:], op=ALU.add, r=[QR0, ident], w=[QR1])
                        yield
                        cur = 1
                        for lv in range(1, 7):
                            Pc = Pb[s][cur]; QRc = QR[s][cur]; Pn = Pb[s][1 - cur]; QRn = QR[s][1 - cur]
                            if lv < 6:
                                MM(pd[:, 0:256], Pc[:], QRc[:, 0:256], r=[Pc, QRc], w=[pd])
                                MM(pd[:, 256:384], QRc[:, 0:128], Pc[:], r=[Pc, QRc], w=[pd])
                                yield
                                OP("act", "copy", QRn[:, 0:128], pd[:, 0:128], r=[pd], w=[QRn])
                                OP("dve", "tensor_tensor", QRn[:, 128:256], QRc[:, 128:256], pd[:, 128:256], op=ALU.add, r=[pd, QRc], w=[QRn])
                                OP("act", "copy", Pn[:], pd[:, 256:384], r=[pd], w=[Pn])
                                yield
                            else:
                                MM(pd[:, 128:256], Pc[:], QRc[:, 128:256], r=[Pc, QRc], w=[pd])
                                yield
                                OP("dve", "tensor_tensor", QRn[:, 128:256], QRc[:, 128:256], pd[:, 128:256], op=ALU.add, r=[pd, QRc], w=[QRn])
                                yield
                            cur = 1 - cur
                        Rf = QR[s][cur]
                        OP("dve", "tensor_scalar", TB[s][:], Rf[:, 128:256], col("beta"), None, op0=ALU.mult, r=[Rf, gates["beta"]], w=[TB[s]])
                        OP("pool", "tensor_scalar", TBG[s][:], Rf[:, 128:256], col("bg"), None, op0=ALU.mult, r=[Rf, gates["bg"]], w=[TBG[s]])
                        OP("pool", "tensor_scalar", kdec[s][slot][:], k_tok[:, i, :], col("ekd"), None, op0=ALU.mult,
                           r=[k_tok.r[i], gates["ekd"]], w=[kdec[s][slot]])
                        yield
                        MM(pd[:, 0:128], TB[s][:], v_tok[:, i, :], r=[TB[s], v_tok.r[i]], w=[pd])
                        MM(pd[:, 128:256], k_tok[:, i, :], TBG[s][:], r=[TBG[s], k_tok.r[i]], w=[pd])
                        yield
                        OP("act", "copy", uw[s][slot][:], pd[:, 0:256], r=[pd], w=[uw[s][slot]])
                        yield

                    def scan(s, i, slot, want_out, stp):
                        j = s * 8 + h
                        Sc = Sst[s][stp % 2]; Sn = Sst[s][(stp + 1) % 2]
                        pS = PS[4 + s]
                        MM(pS[:, 0:128], uw[s][slot][:, 128:256], Sc[:], r=[uw[s][slot], Sc], w=[pS])
                        yield
                        OP("dve", "tensor_tensor", vnew[s][:], uw[s][slot][:, 0:128], pS[:, 0:128], op=ALU.subtract,
                           r=[uw[s][slot], pS], w=[vnew[s]])
                        yield
                        if want_out:
                            MM(pS[:, 128:256], qdT[s][slot][:], Sc[:], start=True, stop=False, r=[qdT[s][slot], Sc], w=[pS])
                            MM(pS[:, 128:256], qkT[s][slot][:], vnew[s][:], start=False, stop=True, r=[qkT[s][slot], vnew[s]], w=[pS])
                        MM(pS[:, 256:384], kdec[s][slot][:], vnew[s][:], r=[kdec[s][slot], vnew[s]], w=[pS])
                        yield
                        if want_out:
                            if s == 0:
                                OP("act", "copy", o_acc[:, i, :], pS[:, 128:256], r=[pS], w=[o_acc.r[i]])
                            else:
                                OP("dve", "tensor_tensor", o_acc[:, i, :], o_acc[:, i, :], pS[:, 128:256], op=ALU.add,
                                   r=[pS, o_acc.r[i]], w=[o_acc.r[i]])
                        OP("dve", "scalar_tensor_tensor", Sn[:], Sc[:], gates["gtot"][:, i, j:j + 1], pS[:, 256:384],
                           op0=ALU.mult, op1=ALU.add, r=[Sc, gates["gtot"], pS], w=[Sn])
                        yield

                    def run_pair(gens):
                        gens = [g for g in gens if g is not None]
                        while gens:
                            for g in list(gens):
                                try:
                                    next(g)
                                except StopIteration:
                                    gens.remove(g)

                    seqF = [16, 17] + list(range(8))
                    seqB = [17, 16] + list(range(15, -1, -1))
                    for s in range(2):
                        OP("pool", "memset", Sst[s][0][:], 0.0, w=[Sst[s][0]])
                    def mk(kind, s_, n_):
                        seq = seqF if s_ == 0 else seqB
                        if n_ < 0 or n_ >= len(seq):
                            return None
                        i_ = seq[n_]
                        if kind == "prep":
                            return prep(s_, i_, n_ % NS, i_ < 8)
                        return scan(s_, i_, n_ % NS, i_ < 8, n_)

                    for n in range(19):
                        run_pair([mk("prep", 0, n), mk("prep", 1, n), mk("scan", 0, n - 1), mk("scan", 1, n - 1)])
                    if h == 0:
                        DBG(o_acc, o_acc[:, 0, :], 128); DBG(o_acc, o_acc[:, 7, :], 128)
                        CHK(4)
                    for i in range(8):
                        OP("act", "activation", sq[:, 0:128], o_acc[:, i, :], AF.Square, accum_out=small[:, 8 + i:9 + i],
                           r=[o_acc.r[i]], w=[sq, small])
                    OP("act", "activation", small[:, 16:24], small[:, 8:16], AF.Sqrt, scale=1.0 / 128, bias=EPS, r=[small], w=[small])
                    OP("dve", "reciprocal", small[:, 24:32], small[:, 16:24], r=[small], w=[small])
                    for i in range(8):
                        OP("dve", "tensor_scalar", o_acc[:, i, :], o_acc[:, i, :], small[:, 24 + i:25 + i], None, op0=ALU.mult,
                           r=[o_acc.r[i], small], w=[o_acc.r[i]])
                    for g in range(2):
                        pb = PS[g]
                        for ii in range(4):
                            i = g * 4 + ii
                            TR(pb[:, ii * 128:(ii + 1) * 128], o_acc[:, i, :], ident[:], r=[o_acc.r[i], ident], w=[pb])
                        OP("dve", "scalar_tensor_tensor", catT[:, 8 + h, g * 512:(g + 1) * 512], pb[:, 0:512], ong[:, 0:1],
                           catT[:, 8 + h, g * 512:(g + 1) * 512], op0=ALU.mult, op1=ALU.mult, r=[pb, ong, catT.r[8 + h]], w=[catT.r[8 + h]])

            CHK(5)
            with scope() as st:
                RW, CW = 32, 80
                NPF = RW * CW
                U = sb(st, "U", [128, NPF]); Ba = sb(st, "Ba", [128, NPF]); Bb = sb(st, "Bb", [128, NPF]); Bc = sb(st, "Bc", [128, NPF])
                pinv = sb(st, "pinv", [128, 4, OWN]); dT = [sb(st, "dT%d" % i, [128, OWN], BF16) for i in range(2)]
                pw = sb(st, "pw", [128, 2, 256], BF16)
                DMA("sp", pinv[:], pinv_d, w=[pinv])
                OP("pool", "memset", U[:], 0.0, w=[U])
                fa = flipc[:, 0:1]; fb = flipc[:, 1:2]

                def v3(tl):
                    return tl[:, 0:NPF].rearrange("p (r c) -> p r c", c=CW)

                def lvl1_cols(src, tmp, dst):
                    s3, t3, d3 = v3(src), v3(tmp), v3(dst)
                    OP("dve", "scalar_tensor_tensor", t3[:, :, 1:CW], s3[:, :, 0:CW - 1], fa, s3[:, :, 1:CW], op0=ALU.mult, op1=ALU.add,
                       r=[src, flipc], w=[tmp])
                    OP("pool", "tensor_copy", t3[:, :, 0:1], s3[:, :, 0:1], r=[src], w=[tmp])
                    OP("dve", "scalar_tensor_tensor", d3[:, :, 0:CW - 1], s3[:, :, 1:CW], fb, t3[:, :, 0:CW - 1], op0=ALU.mult, op1=ALU.add,
                       r=[src, tmp, flipc], w=[dst])
                    OP("pool", "tensor_copy", d3[:, :, CW - 1:CW], t3[:, :, CW - 1:CW], r=[tmp], w=[dst])

                def lvl_cols(src, dst, d):
                    s3, d3 = v3(src), v3(dst)
                    OP("dve", "tensor_tensor", d3[:, :, d:CW - d], s3[:, :, 2 * d:CW], s3[:, :, 0:CW - 2 * d], op=ALU.add, r=[src], w=[dst])
                    OP("pool", "tensor_copy", d3[:, :, 0:d], s3[:, :, d:2 * d], r=[src], w=[dst])
                    OP("pool", "tensor_copy", d3[:, :, CW - d:CW], s3[:, :, CW - 2 * d:CW - d], r=[src], w=[dst])

                def lvl1_rows(src, tmp, dst):
                    OP("dve", "scalar_tensor_tensor", tmp[:, CW:NPF], src[:, 0:NPF - CW], fa, src[:, CW:NPF], op0=ALU.mult, op1=ALU.add,
                       r=[src, flipc], w=[tmp])
                    OP("pool", "tensor_copy", tmp[:, 0:CW], src[:, 0:CW], r=[src], w=[tmp])
                    OP("dve", "scalar_tensor_tensor", dst[:, 0:NPF - CW], src[:, CW:NPF], fb, tmp[:, 0:NPF - CW], op0=ALU.mult, op1=ALU.add,
                       r=[src, tmp, flipc], w=[dst])
                    OP("pool", "tensor_copy", dst[:, NPF - CW:NPF], tmp[:, NPF - CW:NPF], r=[tmp], w=[dst])

                def lvl_rows(src, dst, d):
                    e = CW * d
                    OP("dve", "tensor_tensor", dst[:, e:NPF - e], src[:, 2 * e:NPF], src[:, 0:NPF - 2 * e], op=ALU.add, r=[src], w=[dst])
                    OP("pool", "tensor_copy", dst[:, 0:e], src[:, e:2 * e], r=[src], w=[dst])
                    OP("pool", "tensor_copy", dst[:, NPF - e:NPF], src[:, NPF - 2 * e:NPF - e], r=[src], w=[dst])

                for ci in range(8):
                    g = ci // 2
                    wb = load_w(ci * 128)
                    for nt in range(3):
                        pb = PS[nt]
                        proj(wb, 128, nt * 512, 512, pb)
                        OP("act", "copy", v3(U)[:, 8 + nt * 8:16 + nt * 8, 8:72], pb[:, 0:512].rearrange("p (r c) -> p r c", c=64), r=[pb], w=[U])
                    lvl1_cols(U, Ba, Bb)
                    cur, oth = Bb, Ba
                    for lv in range(1, g + 1):
                        lvl_cols(cur, oth, 2 ** (lv - 1))
                        cur, oth = oth, cur
                    lvl1_rows(cur, oth, Bc)
                    cur, oth = Bc, oth
                    for lv in range(1, g + 1):
                        lvl_rows(cur, oth, 2 ** (lv - 1))
                        cur, oth = oth, cur
                    dd = dT[ci % 2]
                    ci3 = v3(cur)[:, 8:24, 8:72]
                    OP("dve", "tensor_tensor", ci3, ci3, pinv[:, g, :].rearrange("p (r c) -> p r c", c=64), op=ALU.mult, r=[cur, pinv], w=[cur])
                    OP("dve", "tensor_tensor", dd[:].rearrange("p (r c) -> p r c", c=64), ci3, v3(U)[:, 8:24, 8:72], op=ALU.subtract, r=[cur, U], w=[dd])
                    if ci % 2 == 1:
                        DMA("pool", pw[:], poolw_d[g].rearrange("(k p) n -> p k n", p=128), w=[pw])
                        for co in range(2):
                            for nt in range(2):
                                pb = PS[4 + nt]
                                for k in range(2):
                                    MM(pb[:, 0:512], pw[:, k, co * 128:(co + 1) * 128], dT[k][:, nt * 512:(nt + 1) * 512],
                                       start=(k == 0), stop=(k == 1), r=[pw, dT[k]], w=[pb])
                                OP("act", "activation", catT[:, 2 * g + co, nt * 512:(nt + 1) * 512], pb[:, 0:512], AF.Identity,
                                   scale=psT[:, 2 * g + co:2 * g + co + 1], r=[pb, psT], w=[catT.r[2 * g + co]])

        with scope() as pm:
            xacc = sb(pm, "xacc", [128, 8, D], n=8)
            rowb = sb(pm, "rowb", [128, D])
            bl = sb(pm, "bl", [128, 128])
            lg = sb(pm, "lg", [128, 8, 36]); gatem = sb(pm, "gatem", [128, 8, 32])

            def bcast_rows(dst, colsrc, t, j0):
                for kg in range(4):
                    pb = PS[kg]
                    for kk in range(4):
                        k = kg * 4 + kk
                        OP("pool", "tensor_copy", bl[:], modT[:, j0 + k, t:t + 1].to_broadcast([128, 128]), r=[modT], w=[bl])
                        MM(pb[:, kk * 128:(kk + 1) * 128], bl[:], ident[:], r=[bl, ident], w=[pb])
                    OP("act", "copy", dst[:, kg * 512:(kg + 1) * 512], pb[:, 0:512], r=[pb], w=[dst])

            for i in range(8):
                DMA("sp", xacc[:, i, :], x_d[i * 128:(i + 1) * 128, :], w=[xacc.r[i]])
            with scope() as st:
                wo = [sb(st, "wo%d" % i, [128, 16, 512], BF16) for i in range(2)]
                tmpw = sb(st, "tmpw", [128, 512])
                bcast_rows(rowb, None, 0, 32)
                wov = wout_d.rearrange("(k p) n -> p k n", p=128)
                for dq in range(4):
                    wb = wo[dq % 2]
                    DMA("pool", wb[:], wov[:, :, dq * 512:(dq + 1) * 512], w=[wb])
                    for i in range(8):
                        pb = PS[4 + i % 2]
                        for k in range(16):
                            MM(pb[:, 0:512], catT[:, k, i * 128:(i + 1) * 128], wb[:, k, :], start=(k == 0), stop=(k == 15),
                               r=[catT.r[k], wb], w=[pb])
                        OP("dve", "tensor_tensor", tmpw[:], pb[:, 0:512], rowb[:, dq * 512:(dq + 1) * 512], op=ALU.mult, r=[pb, rowb], w=[tmpw])
                        OP("dve", "tensor_tensor", xacc[:, i, dq * 512:(dq + 1) * 512], xacc[:, i, dq * 512:(dq + 1) * 512], tmpw[:],
                           op=ALU.add, r=[tmpw, xacc.r[i]], w=[xacc.r[i]])
            DBG(xacc, xacc[:, 0, 0:512], 512)
            CHK(6)
            h2T = catT
            with scope() as st:
                a2b = sb(st, "a2b", [128, D]); sh2b = sb(st, "sh2b", [128, D]); h2f = sb(st, "h2f", [128, D])
                h2Tf = sb(st, "h2Tf", [128, 512]); wr = sb(st, "wr", [128, 16, 36]); brow = sb(st, "brow", [128, 36])
                junk = sb(st, "junk2", [128, D], BF16)
                DMA("sp", wr[:], wr_d.rearrange("(k p) n -> p k n", p=128), w=[wr]); DMA("sp", brow[:], brow_d, w=[brow])
                DMA("sp", h2f[:], g2_d, w=[h2f])
                bcast_rows(a2b, None, 0, 64)
                OP("dve", "scalar_tensor_tensor", a2b[:], a2b[:], 1.0, h2f[:], op0=ALU.add, op1=ALU.mult, r=[a2b, h2f], w=[a2b])
                bcast_rows(sh2b, None, 0, 48)
                for i in range(8):
                    c0 = small[:, 32:33]; c1 = small[:, 33:34]; c2 = small[:, 34:35]
                    OP("act", "activation", junk[:], xacc[:, i, :], AF.Square, accum_out=c0, r=[xacc.r[i]], w=[junk, small])
                    OP("act", "activation", c1, c0, AF.Sqrt, scale=1.0 / D, bias=EPS, r=[small], w=[small])
                    OP("dve", "reciprocal", c2, c1, r=[small], w=[small])
                    OP("dve", "scalar_tensor_tensor", h2f[:], xacc[:, i, :], c2, a2b[:], op0=ALU.mult, op1=ALU.mult,
                       r=[xacc.r[i], small, a2b], w=[h2f])
                    OP("dve", "tensor_tensor", h2f[:], h2f[:], sh2b[:], op=ALU.add, r=[h2f, sh2b], w=[h2f])
                    plg = PS[7]
                    for kg in range(4):
                        pb = PS[kg]
                        for kk in range(4):
                            k = kg * 4 + kk
                            TR(pb[:, kk * 128:(kk + 1) * 128], h2f[:, k * 128:(k + 1) * 128], ident[:], r=[h2f, ident], w=[pb])
                        OP("act", "copy", h2Tf[:], pb[:, 0:512], r=[pb], w=[h2Tf])
                        OP("dve", "tensor_copy", h2T[:, kg * 4:(kg + 1) * 4, i * 128:(i + 1) * 128],
                           pb[:, 0:512].rearrange("p (k n) -> p k n", n=128), r=[pb], w=h2T.r[kg * 4:(kg + 1) * 4])
                        for kk in range(4):
                            k = kg * 4 + kk
                            MM(plg[:, 0:36], h2Tf[:, kk * 128:(kk + 1) * 128], wr[:, k, :], start=(k == 0), stop=(k == 15), r=[h2Tf, wr], w=[plg])
                    OP("dve", "tensor_tensor", lg[:, i, :], plg[:, 0:36], brow[:], op=ALU.add, r=[plg, brow], w=[lg])
                elm = sb(st, "elm", [128, 32]); oh = sb(st, "oh", [128, 64]); top8 = sb(st, "top8", [128, 8]); scr = sb(st, "scr", [128, 16])
                for i in range(8):
                    gl = lg[:, i, 0:4]
                    S_ = lambda a, b=None: scr[:, a:(a + 1 if b is None else b)]
                    OP("dve", "tensor_reduce", S_(0), gl, axis=AX.X, op=ALU.max, r=[lg], w=[scr])
                    OP("dve", "tensor_scalar", S_(1), S_(0), -1.0, None, op0=ALU.mult, r=[scr], w=[scr])
                    OP("dve", "tensor_scalar", S_(4, 8), gl, S_(0), None, op0=ALU.is_equal, r=[lg, scr], w=[scr])
                    OP("act", "activation", S_(8, 12), gl, AF.Exp, bias=S_(1), accum_out=S_(2), r=[lg, scr], w=[scr])
                    OP("dve", "reciprocal", S_(3), S_(2), r=[scr], w=[scr])
                    OP("dve", "tensor_scalar", S_(4, 8), S_(4, 8), 1.0, 1e30, op0=ALU.subtract, op1=ALU.mult, r=[scr], w=[scr])
                    for g in range(4):
                        OP("dve", "tensor_scalar", elm[:, g * 8:(g + 1) * 8], lg[:, i, 4 + g * 8:12 + g * 8], S_(4 + g), None, op0=ALU.add,
                           r=[lg, scr], w=[elm])
                    OP("dve", "max", out=top8[:], in_=elm[:], r=[elm], w=[top8])
                    OP("dve", "tensor_scalar", oh[:, 0:32], elm[:], top8[:, 0:1], None, op0=ALU.is_equal, r=[elm, top8], w=[oh])
                    OP("dve", "tensor_scalar", oh[:, 32:64], elm[:], top8[:, 1:2], None, op0=ALU.is_equal, r=[elm, top8], w=[oh])
                    OP("dve", "tensor_scalar", S_(12), top8[:, 0:1], -1.0, None, op0=ALU.mult, r=[top8], w=[scr])
                    OP("act", "activation", S_(13), top8[:, 1:2], AF.Exp, bias=S_(12), r=[top8, scr], w=[scr])
                    OP("dve", "tensor_scalar", S_(13), S_(13), 1.0, None, op0=ALU.add, r=[scr], w=[scr])
                    OP("dve", "reciprocal", S_(14), S_(13), r=[scr], w=[scr])
                    OP("dve", "tensor_tensor", S_(14), S_(14), S_(3), op=ALU.mult, r=[scr], w=[scr])
                    OP("dve", "tensor_tensor", S_(15), S_(3), S_(14), op=ALU.subtract, r=[scr], w=[scr])
                    OP("dve", "tensor_scalar", gatem[:, i, :], oh[:, 0:32], S_(14), None, op0=ALU.mult, r=[oh, scr], w=[gatem])
                    OP("dve", "scalar_tensor_tensor", gatem[:, i, :], oh[:, 32:64], S_(15), gatem[:, i, :], op0=ALU.mult, op1=ALU.add,
                       r=[oh, scr, gatem], w=[gatem])
            DBG(lg, lg[:, 0, :], 36); DBG(gatem, gatem[:, 0, :], 32)
            CHK(7)

            with scope() as st:
                NW = 4
                wring = [sb(st, "wring%d" % i, [128, 16, 256], BF16) for i in range(NW)]
                w2b = sb(st, "w2b", [128, 8, D], BF16)
                actT = sb(st, "actT", [128, 8, OWN], BF16, n=8)
                sg = [sb(st, "sg%d" % i, [128, 512]) for i in range(2)]
                ytmp = [sb(st, "ytmp%d" % i, [128, 512]) for i in range(2)]
                bcast_rows(rowb, None, 0, 80)
                pieces = []
                for e_ in range(NE):
                    w1v_ = w1_d[e_].rearrange("(k p) n -> p k n", p=128)
                    w3v_ = w3_d[e_].rearrange("(k p) n -> p k n", p=128)
                    for q_ in range(4):
                        pieces.append(w1v_[:, :, q_ * 256:(q_ + 1) * 256])
                        pieces.append(w3v_[:, :, q_ * 256:(q_ + 1) * 256])
                issued = [0]
                used = [0]

                def wload():
                    idx = used[0]
                    used[0] += 1
                    while issued[0] < min(len(pieces), idx + NW - 1):
                        p_ = issued[0]
                        issued[0] += 1
                        DMA("pool", wring[p_ % NW][:], pieces[p_], w=[wring[p_ % NW]])
                    return wring[idx % NW]

                hcnt = 0
                ycnt = 0
                for e in range(NE):
                    DMA("pool", w2b[:], w2_d[e].rearrange("(k p) n -> p k n", p=128), w=[w2b])
                    for q in range(4):
                        w1p = wload()
                        w3p = wload()
                        for f2 in range(2):
                            fc = q * 2 + f2
                            for nt in range(2):
                                ph1 = PS[hcnt % 2]; ph3 = PS[2 + hcnt % 2]; sgb = sg[hcnt % 2]
                                hcnt += 1
                                for dk in range(16):
                                    MM(ph1[:, 0:512], w1p[:, dk, f2 * 128:(f2 + 1) * 128], h2T[:, dk, nt * 512:(nt + 1) * 512],
                                       start=(dk == 0), stop=(dk == 15), r=[w1p, h2T.r[dk]], w=[ph1])
                                for dk in range(16):
                                    MM(ph3[:, 0:512], w3p[:, dk, f2 * 128:(f2 + 1) * 128], h2T[:, dk, nt * 512:(nt + 1) * 512],
                                       start=(dk == 0), stop=(dk == 15), r=[w3p, h2T.r[dk]], w=[ph3])
                                OP("act", "activation", sgb[:], ph1[:, 0:512], AF.Silu, r=[ph1], w=[sgb])
                                OP("dve", "tensor_tensor", actT[:, fc, nt * 512:(nt + 1) * 512], sgb[:], ph3[:, 0:512], op=ALU.mult,
                                   r=[sgb, ph3], w=[actT.r[fc]])
                    for i in range(8):
                        for dq in range(4):
                            py = PS[4 + ycnt % 3]; yt = ytmp[ycnt % 2]
                            ycnt += 1
                            for fc in range(8):
                                MM(py[:, 0:512], actT[:, fc, i * 128:(i + 1) * 128], w2b[:, fc, dq * 512:(dq + 1) * 512],
                                   start=(fc == 0), stop=(fc == 7), r=[actT.r[fc], w2b], w=[py])
                            OP("act", "activation", yt[:], py[:, 0:512], AF.Identity, scale=gatem[:, i, e:e + 1], r=[py, gatem], w=[yt])
                            OP("dve", "tensor_tensor", yt[:], yt[:], rowb[:, dq * 512:(dq + 1) * 512], op=ALU.mult, r=[yt, rowb], w=[yt])
                            OP("pool", "tensor_tensor", xacc[:, i, dq * 512:(dq + 1) * 512], xacc[:, i, dq * 512:(dq + 1) * 512], yt[:],
                               op=ALU.add, r=[yt, xacc.r[i]], w=[xacc.r[i]])

            DBG(xacc, xacc[:, 0, 0:512], 512)
            with scope() as st:
                fgb = sb(st, "fgb", [128, D]); junk = sb(st, "junk3", [128, D], BF16)
                ot = [sb(st, "ot%d" % i, [128, D]) for i in range(2)]
                DMA("sp", fgb[:], fg_d, w=[fgb])
                for i in range(8):
                    c0 = small[:, 40:41]; c1 = small[:, 41:42]; c2 = small[:, 42:43]
                    OP("act", "activation", junk[:], xacc[:, i, :], AF.Square, accum_out=c0, r=[xacc.r[i]], w=[junk, small])
                    OP("act", "activation", c1, c0, AF.Sqrt, scale=1.0 / D, bias=EPS, r=[small], w=[small])
                    OP("dve", "reciprocal", c2, c1, r=[small], w=[small])
                    OP("dve", "scalar_tensor_tensor", ot[i % 2][:], xacc[:, i, :], c2, fgb[:], op0=ALU.mult, op1=ALU.mult,
                       r=[xacc.r[i], small, fgb], w=[ot[i % 2]])
                    final_ops.append(DMA("sp", out_d[i * 128:(i + 1) * 128, :], ot[i % 2][:], r=[ot[i % 2]], w=[out_dram]))

        pass
    except _Stop:
        pass
    P.emit(final_ops)
    return nc, P.stats


def _fm(v, n):
    return np.ascontiguousarray(np.asarray(v, np.float32).reshape(n, 128).T)


def _pinv(rev):
    out = np.zeros((4, 16, 64), np.float32)
    for g, w in enumerate((2, 4, 8, 16)):
        lo = w // 2
        hi = w - lo

        def cnt(n):
            i = np.arange(n)
            return np.clip(i + hi, 0, n) - np.clip(i - lo, 0, n)
        nr = cnt(32).astype(np.float32)
        ncn = cnt(64).astype(np.float32)
        if rev:
            nr = nr[::-1]
            ncn = ncn[::-1]
        out[g] = 1.0 / (nr[:16, None] * ncn[None, :])
    return np.ascontiguousarray(np.broadcast_to(out.reshape(1, 4, OWN), (128, 4, OWN)))


def prep_core(inp, b, th):
    rev = th == 1
    f32 = lambda a: np.ascontiguousarray(np.asarray(a, np.float32))
    x = inp["x"][b]
    ctx = inp["ctx"][b]
    if rev:
        x = x[::-1]
        ctx = ctx[::-1]
    m = {"x": f32(x), "ctx": f32(ctx)}
    cc = np.zeros((128, 16, 2), np.float32)
    cc[:, :, 0] = _fm(inp["c"][b], 16)
    cc[:, :, 1] = _fm(inp["c_ctx"], 16)
    m["cc"] = cc
    m["w_mod"] = f32(inp["w_mod"][0])
    m["bmodT"] = _fm(inp["b_mod"][0], 96)
    m["g1T"] = _fm(inp["norm1_g"][0], 16)
    cols = list(range(1024))
    for h in range(8):
        for base in (1024, 2048, 3072, 4096):
            cols += list(range(base + h * 128, base + (h + 1) * 128))
    AB0 = 5120
    bf, bb, af, ab_ = [list(range(AB0 + 8 * i, AB0 + 8 * i + 8)) for i in range(4)]
    cols += (bb + bf + ab_ + af) if rev else (bf + bb + af + ab_)
    m["w_in"] = f32(inp["w_in"][0][:, cols])
    cwa = np.asarray(inp["conv_w"][0], np.float32)
    if rev:
        cwa = cwa[::-1]
    cw = cwa.reshape(5, 3, 8, 128).transpose(3, 2, 1, 0)
    m["cw"] = f32(cw)
    s0a, s1a = (inp["a_log_b"][0], inp["a_log_f"][0]) if rev else (inp["a_log_f"][0], inp["a_log_b"][0])
    s0d, s1d = (inp["dt_bias_b"][0], inp["dt_bias_f"][0]) if rev else (inp["dt_bias_f"][0], inp["dt_bias_b"][0])
    m["alog_rep"] = f32(np.broadcast_to(np.concatenate([s0a, s1a])[None, None, :], (128, 18, 16)))
    m["dtb_rep"] = f32(np.broadcast_to(np.concatenate([s0d, s1d])[None, None, :], (128, 18, 16)))
    m["ong"] = f32(np.asarray(inp["out_norm_g"][0]).reshape(128, 1))
    m["pool_w"] = f32(inp["pool_w"][0])
    m["psT"] = _fm(inp["pool_scale"][0], 8)
    m["pinv"] = _pinv(rev)
    m["flipc"] = f32(np.broadcast_to(np.array([[0.0, 1.0]] if rev else [[1.0, 0.0]], np.float32), (128, 2)))
    m["w_out"] = f32(inp["w_out"][0])
    m["g2_bc"] = f32(np.broadcast_to(np.asarray(inp["norm2_g"][0])[None, :], (128, D)))
    m["fg_bc"] = f32(np.broadcast_to(np.asarray(inp["final_g"])[None, :], (128, D)))
    m["wr"] = f32(np.concatenate([inp["w_grp"][0], inp["w_rt"][0]], axis=1))
    m["brow"] = f32(np.broadcast_to(np.concatenate([inp["b_grp"][0], inp["b_rt"][0]])[None, :], (128, 36)))
    m["w1"] = f32(inp["w1"][0])
    m["w3"] = f32(inp["w3"][0])
    m["w2"] = f32(inp["w2"][0])
    return m


_CACHE = {}


def kernel(**inputs):
    inp = {k: np.asarray(v) for k, v in inputs.items()}
    if "nc" not in _CACHE:
        _CACHE["nc"] = build()[0]
    nc = _CACHE["nc"]
    shared = {}
    in_maps = []
    for core in range(8):
        b, th = core // 2, core % 2
        m = prep_core(inp, b, th)
        for k in ("w_mod", "w_out", "w1", "w3", "w2", "pool_w", "wr"):
            if k in shared:
                m[k] = shared[k]
            else:
                shared[k] = m[k]
        in_maps.append(m)
    res = run_bass_kernel_spmd(nc, in_maps, core_ids=list(range(8)))
    out = np.zeros((4, T, D), np.float32)
    for core in range(8):
        b, th = core // 2, core % 2
        o = np.asarray(res.results[core]["out"], np.float32)
        if th == 0:
            out[b, 0:OWN] = o
        else:
            out[b, T - OWN:T] = o[::-1]
    return out
```
